# Optimizing a Trainium2 kernel written in Bass

```python
import math
import jax, jax.numpy as jnp
from jax import lax
import numpy as np

D_MODEL = 1024
BATCH = 4
SEQ = 4096
DEPTH = 2

GRID_W = 64
CTX_LEN = 256
EPS = 1e-6
NEG = -1e30

D_MIX = D_MODEL
FOURIER_GROUPS = 4
FOURIER_CH = 64
D_FOURIER = FOURIER_GROUPS * FOURIER_CH
HEAD_DIM = 64
ATT_HEADS = 8
ATT_KV_HEADS = 2
ATT_GROUP = ATT_HEADS // ATT_KV_HEADS
D_ATT = ATT_HEADS * HEAD_DIM
D_KV = ATT_KV_HEADS * HEAD_DIM
WINDOW = 128
ATT_BLOCK = 128
ROPE_BASE = 10000.0
MLSTM_HEADS = 4
MLSTM_DIM = 64
D_MLSTM = MLSTM_HEADS * MLSTM_DIM
MLSTM_CHUNK = 64
QK_CONV = 3
D_FF = 2816
N_MOD = 9

SPLIT_SIZES = (D_FOURIER, D_ATT, D_KV, D_KV, D_MLSTM, D_MLSTM, D_MLSTM, D_MLSTM, 2 * MLSTM_HEADS, 2 * MLSTM_HEADS)
D_IN = D_FOURIER + D_ATT + 2 * D_KV + 4 * D_MLSTM + 4 * MLSTM_HEADS

kernel_name = "hybrid_fourier_swa_mlstm_macaron_dit"

F32 = jnp.float32


def rmsnorm(t, g):
    tf = t.astype(F32)
    y = tf * lax.rsqrt(jnp.mean(tf * tf, axis=-1, keepdims=True) + EPS)
    return (y * g.astype(F32)).astype(t.dtype)


def modulate(h, shift, scale):
    return h * (1 + scale) + shift


def swiglu(h, w_in, w_out):
    g, u = jnp.split(h @ w_in, 2, axis=-1)
    return (jax.nn.silu(g) * u) @ w_out


def split_columns(u):
    idx = []
    total = 0
    for s in SPLIT_SIZES[:-1]:
        total += s
        idx.append(total)
    return jnp.split(u, idx, axis=-1)


def axial_rope(n):
    rows = n // GRID_W
    row = jnp.broadcast_to(jnp.arange(rows, dtype=F32)[:, None], (rows, GRID_W)).reshape(n)
    col = jnp.broadcast_to(jnp.arange(GRID_W, dtype=F32)[None, :], (rows, GRID_W)).reshape(n)
    nf = HEAD_DIM // 4
    inv = ROPE_BASE ** (-jnp.arange(nf, dtype=F32) / nf)
    ar = row[:, None] * inv
    ac = col[:, None] * inv
    ang = jnp.concatenate([ar, ar, ac, ac], axis=-1)
    return jnp.cos(ang), jnp.sin(ang)


def apply_rope(t, cos, sin):
    t = t.astype(F32)
    t1, t2, t3, t4 = jnp.split(t, 4, axis=-1)
    rot = jnp.concatenate([-t2, t1, -t4, t3], axis=-1)
    return t * cos[:, None, :] + rot * sin[:, None, :]


def fourier_mix(u, w):
    b, n, _ = u.shape
    ug = u.astype(F32).reshape(b, n, FOURIER_GROUPS, FOURIER_CH)
    f = jnp.fft.fftn(ug, axes=(1, 3), norm="ortho").real
    y = jnp.einsum('bngc,gce->bnge', f, w.astype(F32))
    return y.reshape(b, n, D_FOURIER).astype(u.dtype)


def band(t, nb):
    b = t.shape[0]
    pad = jnp.zeros((b, ATT_BLOCK) + t.shape[2:], t.dtype)
    tp = jnp.concatenate([pad, t, pad], axis=1).reshape((b, nb + 2, ATT_BLOCK) + t.shape[2:])
    return jnp.concatenate([tp[:, :-2], tp[:, 1:-1], tp[:, 2:]], axis=2)


def window_attention(q, k, v, kc, vc, sink):
    b, n, _, d = q.shape
    nb = n // ATT_BLOCK
    scale = d ** -0.5
    qb = q.astype(F32).reshape(b, nb, ATT_BLOCK, ATT_KV_HEADS, ATT_GROUP, d)
    kband = band(k.astype(F32), nb)
    vband = band(v.astype(F32), nb)
    s_loc = jnp.einsum('bnqkgd,bnskd->bnkgqs', qb, kband) * scale
    qi = jnp.arange(ATT_BLOCK)[:, None]
    sj = jnp.arange(3 * ATT_BLOCK)[None, :]
    kglob = jnp.arange(nb)[:, None, None] * ATT_BLOCK + sj[None] - ATT_BLOCK
    valid = (jnp.abs(sj - ATT_BLOCK - qi) <= WINDOW)[None] & (kglob >= 0) & (kglob < n)
    s_loc = jnp.where(valid[None, :, None, None], s_loc, NEG)
    s_ctx = jnp.einsum('bnqkgd,bckd->bnkgqc', qb, kc.astype(F32)) * scale
    sink_l = jnp.broadcast_to(sink.astype(F32).reshape(ATT_KV_HEADS, ATT_GROUP)[None, None, :, :, None, None],
                              s_loc.shape[:-1] + (1,))
    p = jax.nn.softmax(jnp.concatenate([s_loc, s_ctx, sink_l], axis=-1), axis=-1)
    nl = 3 * ATT_BLOCK
    nc = kc.shape[1]
    out = (jnp.einsum('bnkgqs,bnskd->bnqkgd', p[..., :nl], vband)
           + jnp.einsum('bnkgqc,bckd->bnqkgd', p[..., nl:nl + nc], vc.astype(F32)))
    return out.reshape(b, n, D_ATT)


def context_attention(qc, kc, vc, sink):
    b, cl, _, d = qc.shape
    qg = qc.astype(F32).reshape(b, cl, ATT_KV_HEADS, ATT_GROUP, d)
    s = jnp.einsum('bqkgd,bskd->bkgqs', qg, kc.astype(F32)) * (d ** -0.5)
    sink_c = jnp.broadcast_to(sink.astype(F32).reshape(ATT_KV_HEADS, ATT_GROUP)[None, :, :, None, None],
                              s.shape[:-1] + (1,))
    p = jax.nn.softmax(jnp.concatenate([s, sink_c], axis=-1), axis=-1)
    out = jnp.einsum('bkgqs,bskd->bqkgd', p[..., :cl], vc.astype(F32))
    return out.reshape(b, cl, D_ATT)


def centred_dwconv(t, w):
    kw = w.shape[0]
    half = kw // 2
    n = t.shape[1]
    tp = jnp.pad(t, ((0, 0), (half, half), (0, 0)))
    out = tp[:, 0:n] * w[0]
    for j in range(1, kw):
        out = out + tp[:, j:j + n] * w[j]
    return out


def mlstm_prepare(mq, mk, mv, mi, mf, conv_w, b_i, b_f):
    b, n, _ = mq.shape
    qk = jax.nn.silu(centred_dwconv(jnp.concatenate([mq, mk], axis=-1), conv_w))
    q, k = jnp.split(qk, 2, axis=-1)

    def heads(t):
        return t.astype(F32).reshape(b, n, MLSTM_HEADS, MLSTM_DIM).transpose(0, 2, 1, 3)

    q = heads(q)
    k = heads(k) * (MLSTM_DIM ** -0.5)
    v = heads(mv)
    li = (mi.astype(F32).reshape(b, n, 2, MLSTM_HEADS) + b_i.astype(F32)).transpose(2, 0, 3, 1)
    lf = jax.nn.log_sigmoid(mf.astype(F32).reshape(b, n, 2, MLSTM_HEADS) + b_f.astype(F32)).transpose(2, 0, 3, 1)
    return q, k, v, li, lf


def mlstm_zero_state(b):
    return (jnp.zeros((b, MLSTM_HEADS, MLSTM_DIM, MLSTM_DIM), F32),
            jnp.zeros((b, MLSTM_HEADS, MLSTM_DIM), F32),
            jnp.zeros((b, MLSTM_HEADS), F32))


def mlstm_chunkwise(q, k, v, li, lf, state):
    b, h, n, d = q.shape
    L = MLSTM_CHUNK
    nc = n // L

    def to_chunks(a):
        return jnp.moveaxis(a.reshape((b, h, nc, L) + a.shape[3:]), 2, 0)

    xs = (to_chunks(q), to_chunks(k), to_chunks(v), to_chunks(li), to_chunks(lf))
    causal = jnp.tril(jnp.ones((L, L), dtype=bool))

    def step(carry, inp):
        C, nv, m = carry
        qc, kc, vc, lic, lfc = inp
        bcum = jnp.cumsum(lfc, axis=-1)
        dmat = bcum[..., :, None] - bcum[..., None, :] + lic[..., None, :]
        dmat = jnp.where(causal, dmat, -jnp.inf)
        g_prev = bcum + m[..., None]
        m_t = jnp.maximum(g_prev, jnp.max(dmat, axis=-1))
        w_intra = jnp.exp(dmat - m_t[..., None])
        w_prev = jnp.exp(g_prev - m_t)
        qk = jnp.einsum('bhtd,bhsd->bhts', qc, kc) * w_intra
        num = (jnp.einsum('bhts,bhsv->bhtv', qk, vc)
               + w_prev[..., None] * jnp.einsum('bhvk,bhtk->bhtv', C, qc))
        den = qk.sum(axis=-1) + w_prev * jnp.einsum('bhtk,bhk->bht', qc, nv)
        hout = num / jnp.maximum(jnp.abs(den), jnp.exp(-m_t))[..., None]
        b_last = bcum[..., -1]
        dlast = b_last[..., None] - bcum + lic
        m_new = jnp.maximum(b_last + m, jnp.max(dlast, axis=-1))
        w_s = jnp.exp(dlast - m_new[..., None])
        decay = jnp.exp(b_last + m - m_new)
        C_new = decay[..., None, None] * C + jnp.einsum('bhs,bhsv,bhsk->bhvk', w_s, vc, kc)
        n_new = decay[..., None] * nv + jnp.einsum('bhs,bhsk->bhk', w_s, kc)
        return (C_new, n_new, m_new), hout

    state, hs = lax.scan(step, state, xs)
    hs = jnp.moveaxis(hs, 0, 2).reshape(b, h, n, d)
    return hs, state


def mlstm_bidir(q, k, v, li, lf, init_fwd, init_bwd):
    h_f, s_f = mlstm_chunkwise(q, k, v, li[0], lf[0], init_fwd)
    h_b, s_b = mlstm_chunkwise(jnp.flip(q, 2), jnp.flip(k, 2), jnp.flip(v, 2),
                               jnp.flip(li[1], -1), jnp.flip(lf[1], -1), init_bwd)
    return h_f + jnp.flip(h_b, 2), s_f, s_b


def mlstm_output(hsum, mo):
    b, h, n, d = hsum.shape
    hh = hsum.transpose(0, 2, 1, 3).reshape(b, n, h * d)
    return (jax.nn.sigmoid(mo.astype(F32)) * hh).astype(mo.dtype)


def trunk_layer(x, xc, mod_x, mod_c, norm_g, w_ffn_in, w_ffn_out, w_in, w_out, w_fourier,
                sink, conv_qk, b_gate_i, b_gate_f, cos, sin, last):
    mx = jnp.split(mod_x, N_MOD, axis=-1)
    mc = jnp.split(mod_c, N_MOD, axis=-1)
    b, n, _ = x.shape
    cl = xc.shape[1]
    dt = x.dtype

    def ffn_half(t, mods, sub, f):
        h = modulate(rmsnorm(t, norm_g[sub]), mods[3 * sub], mods[3 * sub + 1])
        return t + 0.5 * mods[3 * sub + 2] * swiglu(h, w_ffn_in[f], w_ffn_out[f])

    x = ffn_half(x, mx, 0, 0)
    xc = ffn_half(xc, mc, 0, 0)

    hx = modulate(rmsnorm(x, norm_g[1]), mx[3], mx[4])
    hc = modulate(rmsnorm(xc, norm_g[1]), mc[3], mc[4])
    ax, qx, kx, vx, mqx, mkx, mvx, mox, mix_, mfx = split_columns(hx @ w_in)
    ac, qc, kc, vc, mqc, mkc, mvc, moc, mic, mfc = split_columns(hc @ w_in)
    kc = kc.reshape(b, cl, ATT_KV_HEADS, HEAD_DIM)
    vc = vc.reshape(b, cl, ATT_KV_HEADS, HEAD_DIM)

    zero = mlstm_zero_state(b)
    h_ctx, st_f, st_b = mlstm_bidir(*mlstm_prepare(mqc, mkc, mvc, mic, mfc, conv_qk, b_gate_i, b_gate_f), zero, zero)
    h_lat, _, _ = mlstm_bidir(*mlstm_prepare(mqx, mkx, mvx, mix_, mfx, conv_qk, b_gate_i, b_gate_f), st_f, st_b)

    qr = apply_rope(qx.reshape(b, n, ATT_HEADS, HEAD_DIM), cos, sin)
    kr = apply_rope(kx.reshape(b, n, ATT_KV_HEADS, HEAD_DIM), cos, sin)
    att_x = window_attention(qr, kr, vx.reshape(b, n, ATT_KV_HEADS, HEAD_DIM), kc, vc, sink)

    y = jnp.concatenate([fourier_mix(ax, w_fourier), att_x.astype(dt), mlstm_output(h_lat, mox).astype(dt)], axis=-1) @ w_out
    x = x + mx[5] * y
    x = ffn_half(x, mx, 2, 1)

    if not last:
        att_c = context_attention(qc.reshape(b, cl, ATT_HEADS, HEAD_DIM), kc, vc, sink)
        yc = jnp.concatenate([fourier_mix(ac, w_fourier), att_c.astype(dt), mlstm_output(h_ctx, moc).astype(dt)], axis=-1) @ w_out
        xc = xc + mc[5] * yc
        xc = ffn_half(xc, mc, 2, 1)
    return x, xc


def setup_inputs(seed: int = 0) -> dict:
    key = jax.random.key(seed)
    ks = jax.random.split(key, 20)
    D = D_MODEL
    nrm = jax.random.normal
    x = nrm(ks[0], (BATCH, SEQ, D), F32)
    c = nrm(ks[1], (BATCH, D), F32)
    ctx = nrm(ks[2], (BATCH, CTX_LEN, D), F32)
    c_ctx = nrm(ks[3], (D,), F32)
    w_ada = nrm(ks[4], (DEPTH, D, N_MOD * D), F32) * (0.5 * D ** -0.5)
    b_ada = nrm(ks[5], (DEPTH, N_MOD * D), F32) * 0.01
    norm_g = 1.0 + 0.02 * nrm(ks[6], (DEPTH, 3, D), F32)
    w_ffn_in = nrm(ks[7], (DEPTH, 2, D, 2 * D_FF), F32) * (D ** -0.5)
    w_ffn_out = nrm(ks[8], (DEPTH, 2, D_FF, D), F32) * (D_FF ** -0.5)
    w_in = nrm(ks[9], (DEPTH, D, D_IN), F32) * (D ** -0.5)
    w_out = nrm(ks[10], (DEPTH, D_MIX, D), F32) * (D_MIX ** -0.5)
    w_fourier = nrm(ks[11], (DEPTH, FOURIER_GROUPS, FOURIER_CH, FOURIER_CH), F32) * (FOURIER_CH ** -0.5)
    attn_sink = 0.1 * nrm(ks[12], (DEPTH, ATT_HEADS), F32)
    conv_qk = nrm(ks[13], (DEPTH, QK_CONV, 2 * D_MLSTM), F32) * (QK_CONV ** -0.5)
    b_gate_i = 0.1 * nrm(ks[14], (DEPTH, 2, MLSTM_HEADS), F32)
    b_gate_f = jnp.linspace(3.0, 6.0, MLSTM_HEADS, dtype=F32) + 0.1 * nrm(ks[15], (DEPTH, 2, MLSTM_HEADS), F32)
    g_final = 1.0 + 0.02 * nrm(ks[16], (D,), F32)
    return {"x": x, "c": c, "ctx": ctx, "c_ctx": c_ctx, "w_ada": w_ada, "b_ada": b_ada,
            "norm_g": norm_g, "w_ffn_in": w_ffn_in, "w_ffn_out": w_ffn_out, "w_in": w_in,
            "w_out": w_out, "w_fourier": w_fourier, "attn_sink": attn_sink, "conv_qk": conv_qk,
            "b_gate_i": b_gate_i, "b_gate_f": b_gate_f, "g_final": g_final}


def reference(x, c, ctx, c_ctx, w_ada, b_ada, norm_g, w_ffn_in, w_ffn_out, w_in, w_out, w_fourier,
              attn_sink, conv_qk, b_gate_i, b_gate_f, g_final):
    n = x.shape[1]
    cos, sin = axial_rope(n)
    sc = jax.nn.silu(c)
    scc = jax.nn.silu(c_ctx)
    xc = ctx
    for l in range(DEPTH):
        mod_x = (sc @ w_ada[l] + b_ada[l])[:, None, :]
        mod_c = scc @ w_ada[l] + b_ada[l]
        x, xc = trunk_layer(x, xc, mod_x, mod_c, norm_g[l], w_ffn_in[l], w_ffn_out[l], w_in[l], w_out[l],
                            w_fourier[l], attn_sink[l], conv_qk[l], b_gate_i[l], b_gate_f[l], cos, sin,
                            l == DEPTH - 1)
    return rmsnorm(x, g_final)
```

```python
import numpy as np
import ml_dtypes
import concourse.bass as bass
import concourse.mybir as mybir
from concourse.bass_utils import run_bass_kernel_spmd

F32 = mybir.dt.float32
BF16 = mybir.dt.bfloat16
AF = mybir.ActivationFunctionType
ALU = mybir.AluOpType
AX = mybir.AxisListType

D = 1024
KC = 8
NL = 2048
NCX = 256
T = NL + NCX
DFF = 2816
NJ = 22
DEPTH = 2
EPS = 1e-6
H_LAT = 2
H_HALO = H_LAT + NL
H_CTX = H_HALO + 2
TP = H_CTX + NCX + 2
TILES = [(0, H_LAT, 512, 0), (512, H_LAT + 512, 512, 0), (1024, H_LAT + 1024, 512, 0),
         (1536, H_LAT + 1536, 512, 0), (2048, H_CTX, 256, 1)]
NCH = 18
QUARTERS = [(0, 6), (6, 12), (12, 17), (17, 22)]

C_F = 0
C_Q = 256
C_QP = 768
C_K = 1280
C_KP = 1408
C_V = 1536
C_T2 = 1920
C_END = 2192
C_MQK = 2192
NCOL = 2704


class Prog:
    def __init__(self, nc):
        self.nc = nc
        self.eng = {"pe": nc.tensor, "dve": nc.vector, "act": nc.scalar, "pool": nc.gpsimd, "sp": nc.sync}
        self.sem = {k: nc.alloc_semaphore("prog_" + k) for k in self.eng}
        self.cnt = {k: 0 for k in self.eng}
        self.NS = 8
        self.dsem = {q: [nc.alloc_semaphore(f"dma_{q}_{i}") for i in range(self.NS)] for q in ("sp", "pool", "act")}
        self.dcnt = {q: [0] * self.NS for q in self.dsem}
        self.dnext = {q: 0 for q in self.dsem}
        self.waited = {k: {} for k in self.eng}
        self.last_w = {}
        self.readers = {}
        self.semobj = {}
        for k, s in self.sem.items():
            self.semobj[("e", k)] = s
        for q, lst in self.dsem.items():
            for i, s in enumerate(lst):
                self.semobj[("d", q, i)] = s

    def _wait(self, eng, sid, val):
        if val <= 0:
            return
        if self.waited[eng].get(sid, 0) >= val:
            return
        self.eng[eng].wait_ge(self.semobj[sid], val)
        self.waited[eng][sid] = val

    def _deps(self, eng, R, W, skip_self=False):
        for k in R:
            lw = self.last_w.get(k)
            if lw is not None:
                if not (skip_self and lw[0] == ("e", eng)):
                    self._wait(eng, lw[0], lw[1])
        for k in W:
            lw = self.last_w.get(k)
            if lw is not None:
                if not (skip_self and lw[0] == ("e", eng)):
                    self._wait(eng, lw[0], lw[1])
            for sid, val in self.readers.get(k, {}).items():
                if not (skip_self and sid == ("e", eng)):
                    self._wait(eng, sid, val)

    def _record(self, sid, val, R, W):
        for k in W:
            self.last_w[k] = (sid, val)
            self.readers[k] = {}
        for k in R:
            d = self.readers.setdefault(k, {})
            if d.get(sid, 0) < val:
                d[sid] = val

    def op(self, eng, fn, R=(), W=()):
        self._deps(eng, R, W, skip_self=(eng == "pe"))
        inst = fn(self.eng[eng])
        self.cnt[eng] += 1
        inst.then_inc(self.sem[eng], 1)
        self._record(("e", eng), self.cnt[eng], R, W)
        return inst

    def dma(self, q, out, in_, R=(), W=(), **kw):
        self._deps(q, R, W)
        i = self.dnext[q]
        self.dnext[q] = (i + 1) % self.NS
        sid = ("d", q, i)
        self._wait(q, sid, self.dcnt[q][i])
        self.eng[q].dma_start(out=out, in_=in_, **kw).then_inc(self.dsem[q][i], 16)
        self.dcnt[q][i] += 16
        self._record(sid, self.dcnt[q][i], R, W)

    def cc(self, fn, R=(), W=()):
        self._deps("pool", R, W)
        sem = self.nc.alloc_semaphore(f"cc_{len(self.semobj)}")
        sid = ("c", len(self.semobj))
        self.semobj[sid] = sem
        fn(self.eng["pool"]).then_inc(sem)
        self._record(sid, 1, R, W)

    def barrier(self):
        for e in self.eng:
            for k in self.eng:
                if k != e:
                    self._wait(e, ("e", k), self.cnt[k])
            for q in self.dsem:
                for i in range(self.NS):
                    self._wait(e, ("d", q, i), self.dcnt[q][i])

    def finish(self, keys):
        for k in keys:
            lw = self.last_w.get(k)
            if lw is not None:
                self._wait("sp", lw[0], lw[1])


C_F = 0
C_Q = 256
C_QP = 768
C_K = 1280
C_KP = 1408
C_V = 1536
C_MV = 1664
C_G = 1920
C_MO = 1936
C_MQK = 2192
NCOL = 2704
PBW = 4354
PFW = 526


def build(cfg):
    from contextlib import ExitStack
    nc = bass.Bass("TRN2", target_bir_lowering=False)
    P = Prog(nc)
    dbg = cfg.get("dbg", set())
    uid = [0]

    def din(name, shape, dt=F32):
        return nc.dram_tensor(name, list(shape), dt, kind="ExternalInput").ap()

    def dout(name, shape, dt=F32):
        return nc.dram_tensor(name, list(shape), dt, kind="ExternalOutput").ap()

    def sb(name, shape, dt=F32):
        return nc.alloc_sbuf_tensor(name, list(shape), dt)

    def al(st, shape, dt=F32, name="t"):
        uid[0] += 1
        return st.enter_context(nc.sbuf_tensor(f"{name}_{uid[0]}", list(shape), dt))

    out_keys = []
    minrem = [1 << 30]

    def note_mem():
        minrem[0] = min(minrem[0], nc.sbuf_bytes_remaining)

    def dump(name, src_ap, shape, dt=F32, R=()):
        if name not in dbg:
            return
        d = dout("dbg_" + name, shape, dt)
        P.dma("sp", out=d, in_=src_ap, R=list(R), W=[("dbgout", name)])
        out_keys.append(("dbgout", name))

    x_in = din("xT0", [128, KC, T])
    cT_d = din("cT", [128, KC, 2])
    w_ada = din("w_ada", [DEPTH, D, 9 * D])
    b_adaT = din("b_adaT", [128, DEPTH, 72])
    norm_gT = din("norm_gT", [128, DEPTH, 3, KC])
    g_finT = din("g_finT", [128, KC])
    W1r = din("W1r", [DEPTH, 2, NJ, 128, KC, 256])
    W2 = din("W2", [DEPTH, 2, DFF, D])
    outT = dout("outT", [128, KC, NL])
    w_in_d = din("w_in_p", [DEPTH, D, NCOL])
    convT_d = din("convT", [128, DEPTH, 4, 3])
    bgate_d = din("bgate", [DEPTH, 16])
    sink_d = din("sink", [DEPTH, 8])
    wfour_d = din("w_fourier", [DEPTH, 4, 64, 64])
    wout_d = din("w_out", [DEPTH, D, D])
    cst_f = din("cst_f", [128, 1024])
    cst_b = din("cst_b", [128, 640], BF16)
    rope_d = din("rope", [128, 2, NL])
    dftl_d = din("dft_lat", [2, 32, 128, 2, 2, 512], BF16)
    dftc_d = din("dft_ctx", [128, 2, 2, 256], BF16)
    exch = cfg.get("exch", "input")
    if exch == "cc":
        pb_out = [nc.dram_tensor(f"pb_int{l}", [128, PBW], BF16).ap() for l in range(DEPTH)]
        pf_out = [nc.dram_tensor(f"pf_int{l}", [128, PFW], F32).ap() for l in range(DEPTH)]
        gb_raw = [nc.dram_tensor(f"gb_int{l}", [256, PBW], BF16).ap() for l in range(DEPTH)]
        gf_raw = [nc.dram_tensor(f"gf_int{l}", [256, PFW], F32).ap() for l in range(DEPTH)]
        gb_in = [a.rearrange("(s p) w -> s p w", s=2) for a in gb_raw]
        gf_in = [a.rearrange("(s p) w -> s p w", s=2) for a in gf_raw]
    else:
        pb_out = [dout(f"pb_out{l}", [128, PBW], BF16) for l in range(DEPTH)]
        pf_out = [dout(f"pf_out{l}", [128, PFW], F32) for l in range(DEPTH)]
        gb_in = [din(f"gb_in{l}", [2, 128, PBW], BF16) for l in range(DEPTH)]
        gf_in = [din(f"gf_in{l}", [2, 128, PFW], F32) for l in range(DEPTH)]

    xT = sb("xT", [128, KC, T])
    hxT = sb("hxT", [128, KC, TP], BF16)
    ones_bf = sb("ones_bf", [128, 128], BF16)
    eps_t = sb("eps_t", [128, 1])
    scT = sb("scT", [128, KC, 2])
    badaT = sb("badaT", [128, DEPTH, 72])
    normg = sb("normg", [128, DEPTH, 3, KC])
    gfin = sb("gfin", [128, KC])
    convT = sb("convT_sb", [128, DEPTH, 4, 3])
    modT = sb("modT", [128, 72, 2])
    A_t = sb("A_t", [128, 3, KC, 2])
    G_t = sb("G_t", [128, 3, KC, 2])
    cf = sb("cst_f_sb", [128, 1024])
    cb_ = sb("cst_b_sb", [128, 640], BF16)
    ps = [nc.alloc_psum_tensor(f"ps{i}", [128, 512], F32) for i in range(7)]
    psb = nc.alloc_psum_tensor("psb", [128, 1024], BF16)

    P.op("pool", lambda e: e.memset(ones_bf[:], 1.0), W=["ones_bf"])
    P.op("pool", lambda e: e.memset(eps_t[:], EPS), W=["eps"])
    P.op("pool", lambda e: e.memset(hxT[:], 0.0), W=[("hx", i) for i in range(5)] + ["hxpad"])
    P.dma("sp", out=xT[:, :, 0:1024], in_=x_in[:, :, 0:1024], W=[("x", 0), ("x", 1)])
    P.dma("sp", out=xT[:, :, 1024:T], in_=x_in[:, :, 1024:T], W=[("x", 2), ("x", 3), ("x", 4)])
    P.dma("sp", out=scT[:], in_=cT_d, W=["scT"])
    P.dma("sp", out=badaT[:], in_=b_adaT, W=["bada"])
    P.dma("sp", out=normg[:], in_=norm_gT, W=["normg"])
    P.dma("sp", out=gfin[:], in_=g_finT, W=["gfin"])
    P.dma("sp", out=convT[:], in_=convT_d, W=["convT"])
    P.dma("sp", out=cf[:], in_=cst_f, W=["cst"])
    P.dma("sp", out=cb_[:], in_=cst_b, W=["cst"])
    P.op("act", lambda e: e.activation(out=scT[:], in_=scT[:], func=AF.Silu), R=["scT"], W=["scT"])
    identf = cf[:, 0:128]
    triA = cf[:, 128:256]
    triB = cf[:, 256:384]
    onesf = cf[:, 384:512]
    ccs = cf[:, 512:768]
    selv = cf[:, 768:770]
    notlast = cf[:, 770:771]
    one_t = cf[:, 384:385]
    identb = cb_[:, 0:128]
    m_prev = cb_[:, 128:256]
    m_next = cb_[:, 256:384]
    mh = [cb_[:, 384:512], cb_[:, 512:640]]
    HXALL = [("hx", i) for i in range(5)] + ["hxpad"]

    def a_ap(sub, v, kc):
        return A_t[:, sub, kc, v:v + 1]

    def sh_ap(sub, v, kc):
        return modT[:, (3 * sub) * 8 + kc, v:v + 1]

    def g_ap(sub, v, kc):
        return G_t[:, sub, kc, v:v + 1]

    def chunk_cols(c):
        if c < 16:
            return 128 * c, H_LAT + 128 * c
        return NL + 128 * (c - 16), H_CTX + 128 * (c - 16)

    def mods_phase(l):
        with ExitStack() as st:
            wada_buf = [al(st, [128, KC, 512], F32, "wada") for _ in range(2)]
            note_mem()
            for cg in range(18):
                wb = wada_buf[cg % 2]
                P.dma("sp", out=wb[:], in_=w_ada[l, :, cg * 512:(cg + 1) * 512].rearrange("(kc p) n -> p kc n", p=128),
                      W=[("wada", cg % 2)])
                for cc in range(4):
                    col = cg * 4 + cc
                    for k in range(KC):
                        P.op("pe", lambda e: e.matmul(ps[5][:, col * 2:col * 2 + 2], lhsT=wb[:, k, cc * 128:(cc + 1) * 128],
                                                      rhs=scT[:, k, :], start=(k == 0), stop=(k == KC - 1)),
                             R=[("wada", cg % 2), "scT"], W=[("ps", 5)])
            P.op("dve", lambda e: e.tensor_tensor(out=modT[:], in0=ps[5][:, 0:144].rearrange("p (c v) -> p c v", v=2),
                                                  in1=badaT[:, l, :].unsqueeze(2).to_broadcast([128, 72, 2]), op=ALU.add),
                 R=[("ps", 5), "bada"], W=["modT"])
            for sub in range(3):
                P.op("dve", lambda e: e.scalar_tensor_tensor(
                    out=A_t[:, sub, :, :], in0=modT[:, (3 * sub + 1) * 8:(3 * sub + 2) * 8, :], scalar=1.0,
                    in1=normg[:, l, sub, :].unsqueeze(2).to_broadcast([128, KC, 2]), op0=ALU.add, op1=ALU.mult),
                    R=["modT", "normg"], W=["A_t"])
                P.op("dve", lambda e: e.tensor_scalar(out=G_t[:, sub, :, :], in0=modT[:, (3 * sub + 2) * 8:(3 * sub + 3) * 8, :],
                                                      scalar1=(0.5 if sub != 1 else 1.0), scalar2=None, op0=ALU.mult),
                     R=["modT"], W=["G_t"])
            P.barrier()

    def rstd_tile(ti, NSC):
        sq, rs, rstd = NSC["sq"], NSC["rs"], NSC["rstd"]
        xc, hc, n, v = TILES[ti]
        P.op("act", lambda e: e.activation(out=sq[:, :, 0:n], in_=xT[:, :, xc:xc + n], func=AF.Square),
             R=[("x", ti)], W=["sq"])
        for kc in range(KC):
            P.op("pe", lambda e: e.matmul(ps[6][:, 0:n], lhsT=ones_bf[:], rhs=sq[:, kc, 0:n], start=(kc == 0),
                                          stop=(kc == KC - 1)), R=["sq", "ones_bf"], W=[("ps", 6)])
        P.op("act", lambda e: e.activation(out=rs[:, 0:n], in_=ps[6][:, 0:n], func=AF.Sqrt, bias=eps_t[:], scale=1.0 / D),
             R=[("ps", 6), "eps"], W=["rs"])
        P.op("dve", lambda e: e.reciprocal(out=rstd[:, 0:n], in_=rs[:, 0:n]), R=["rs"], W=["rstd"])

    def norm_scratch(st):
        return {"sq": al(st, [128, KC, 512], BF16, "sq"), "rs": al(st, [128, 512], F32, "rs"),
                "rstd": al(st, [128, 512], F32, "rstd"), "tmp": [al(st, [128, 512], F32, "tmp") for _ in range(2)]}

    def norm_phase(sub):
        with ExitStack() as st:
            NSC = norm_scratch(st)
            note_mem()
            tmp = NSC["tmp"]
            rstd = NSC["rstd"]
            for ti, (xc, hc, n, v) in enumerate(TILES):
                rstd_tile(ti, NSC)
                for kc in range(KC):
                    tb = tmp[kc % 2]
                    P.op("dve", lambda e: e.scalar_tensor_tensor(out=tb[:, 0:n], in0=xT[:, kc, xc:xc + n], scalar=a_ap(sub, v, kc),
                                                                 in1=rstd[:, 0:n], op0=ALU.mult, op1=ALU.mult),
                         R=[("x", ti), "rstd", "A_t"], W=[("tmp", kc % 2)])
                    P.op("act", lambda e: e.activation(out=hxT[:, kc, hc:hc + n], in_=tb[:, 0:n], func=AF.Identity,
                                                       bias=sh_ap(sub, v, kc), scale=1.0),
                         R=[("tmp", kc % 2), "modT"], W=[("hx", ti)])
            P.barrier()

    def ffn_phase(l, f, sub, tiles):
        norm_phase(sub)
        with ExitStack() as st:
            actT = al(st, [128, 6, T], BF16, "actT")
            w1buf = [al(st, [128, KC, 256], BF16, "w1b") for _ in range(3)]
            w2buf = [al(st, [128, 6, D], BF16, "w2b") for _ in range(2)]
            sg = [al(st, [128, 512], F32, "sg") for _ in range(2)]
            note_mem()
            for qi, (j0, j1) in enumerate(QUARTERS):
                nj = j1 - j0
                w2b = w2buf[qi % 2]
                P.dma("pool", out=w2b[:, 0:nj, :], in_=W2[l, f, j0 * 128:j1 * 128, :].rearrange("(j p) c -> p j c", p=128),
                      W=[("w2", qi % 2)])
                for j in range(j0, j1):
                    wb = w1buf[j % 3]
                    P.dma("pool", out=wb[:], in_=W1r[l, f, j], W=[("w1", j % 3)])
                    for ti in tiles:
                        xc, hc, n, v = TILES[ti]
                        gb = ps[ti % 2]
                        ub = ps[2 + ti % 2]
                        for kc in range(KC):
                            P.op("pe", lambda e: e.matmul(gb[:, 0:n], lhsT=wb[:, kc, 0:128], rhs=hxT[:, kc, hc:hc + n],
                                                          start=(kc == 0), stop=(kc == KC - 1)),
                                 R=[("w1", j % 3), ("hx", ti)], W=[("ps", ti % 2)])
                        for kc in range(KC):
                            P.op("pe", lambda e: e.matmul(ub[:, 0:n], lhsT=wb[:, kc, 128:256], rhs=hxT[:, kc, hc:hc + n],
                                                          start=(kc == 0), stop=(kc == KC - 1)),
                                 R=[("w1", j % 3), ("hx", ti)], W=[("ps", 2 + ti % 2)])
                        sgb = sg[ti % 2]
                        P.op("act", lambda e: e.activation(out=sgb[:, 0:n], in_=gb[:, 0:n], func=AF.Silu),
                             R=[("ps", ti % 2)], W=[("sg", ti % 2)])
                        P.op("dve", lambda e: e.tensor_tensor(out=actT[:, j - j0, xc:xc + n], in0=sgb[:, 0:n], in1=ub[:, 0:n],
                                                              op=ALU.mult),
                             R=[("sg", ti % 2), ("ps", 2 + ti % 2)], W=[("act", j - j0, ti)])
                it = 0
                for oc in range(KC):
                    for ti in tiles:
                        xc, hc, n, v = TILES[ti]
                        pbk = 4 + it % 2
                        it += 1
                        ob = ps[pbk]
                        for jj in range(nj):
                            P.op("pe", lambda e: e.matmul(ob[:, 0:n], lhsT=w2b[:, jj, oc * 128:(oc + 1) * 128],
                                                          rhs=actT[:, jj, xc:xc + n], start=(jj == 0), stop=(jj == nj - 1)),
                                 R=[("w2", qi % 2), ("act", jj, ti)], W=[("ps", pbk)])
                        P.op("dve", lambda e: e.scalar_tensor_tensor(out=xT[:, oc, xc:xc + n], in0=ob[:, 0:n],
                                                                     scalar=g_ap(sub, v, oc), in1=xT[:, oc, xc:xc + n],
                                                                     op0=ALU.mult, op1=ALU.add),
                             R=[("ps", pbk), ("x", ti), "G_t"], W=[("x", ti)])
            P.barrier()

    def final_phase():
        with ExitStack() as st:
            NSC = norm_scratch(st)
            obuf = [al(st, [128, KC, 512], F32, "ob") for _ in range(2)]
            note_mem()
            for ti in range(4):
                xc, hc, n, v = TILES[ti]
                rstd_tile(ti, NSC)
                ob = obuf[ti % 2]
                for kc in range(KC):
                    P.op("dve", lambda e: e.scalar_tensor_tensor(out=ob[:, kc, :], in0=xT[:, kc, xc:xc + n],
                                                                 scalar=gfin[:, kc:kc + 1], in1=NSC["rstd"][:, 0:n],
                                                                 op0=ALU.mult, op1=ALU.mult),
                         R=[("x", ti), "rstd", "gfin"], W=[("ob", ti % 2)])
                P.dma("sp", out=outT[:, :, xc:xc + n], in_=ob[:], R=[("ob", ti % 2)], W=[("outT", ti)])
                out_keys.append(("outT", ti))
            P.finish(out_keys)
            P.barrier()

    def mixer_phase(l, stop_after_payload=False):
        last = (l == DEPTH - 1)
        MQ = [("mqk", i) for i in range(5)]
        norm_phase(1)
        wv = w_in_d[l].rearrange("(kc p) n -> p kc n", p=128)
        with ExitStack() as st_mix:
            mlT = al(st_mix, [128, 2, T], BF16, "mlT")
            ufc = al(st_mix, [128, 2, NCX], BF16, "ufc")
            cb_own = al(st_mix, [128, 4], F32, "cbown")
            prelast = al(st_mix, [128, 4], F32, "prelast")
            with ExitStack() as st_ml:
                mqkT = al(st_ml, [128, 4, T], BF16, "mqkT")
                mv_aug = al(st_ml, [128, NCH, 4, 65], BF16, "mvaug")
                gat = al(st_ml, [128, 10, NCH, 8], F32, "gat")
                li, zf, lf, bcum, btot, eq, ek, ev, edec, gtmp = [gat[:, i, :, :] for i in range(10)]
                with ExitStack() as st:
                    wraw = al(st, [128, KC, 512], BF16, "wraw")
                    rawsb = al(st, [128, TP], F32, "rawsb")
                    cvt = al(st, [128, TP], F32, "cvt")
                    note_mem()
                    P.dma("pool", out=wraw[:], in_=wv[:, :, C_MQK:C_MQK + 512], W=["wraw"])
                    for c in range(4):
                        P.op("pool", lambda e: e.memset(rawsb[:], 0.0), W=["rawsb"])
                        for ti, (xc, hc, n, v) in enumerate(TILES):
                            pb_ = ps[ti % 2]
                            for kc in range(KC):
                                P.op("pe", lambda e: e.matmul(pb_[:, 0:n], lhsT=wraw[:, kc, c * 128:(c + 1) * 128],
                                                              rhs=hxT[:, kc, hc:hc + n], start=(kc == 0), stop=(kc == KC - 1)),
                                     R=["wraw"] + HXALL, W=[("ps", ti % 2)])
                            P.op("act", lambda e: e.activation(out=rawsb[:, hc:hc + n], in_=pb_[:, 0:n], func=AF.Identity), R=[("ps", ti % 2)], W=["rawsb"])
                        P.op("dve", lambda e: e.tensor_scalar(out=cb_own[:, c:c + 1], in0=rawsb[:, H_LAT + NL - 1:H_LAT + NL],
                                                              scalar1=convT[:, l, c, 0:1], scalar2=None, op0=ALU.mult),
                             R=["rawsb", "convT"], W=["cb_own"])
                        W_ = TP - 2
                        P.op("dve", lambda e: e.tensor_scalar(out=cvt[:, 1:1 + W_], in0=rawsb[:, 0:W_], scalar1=convT[:, l, c, 0:1],
                                                              scalar2=None, op0=ALU.mult), R=["rawsb", "convT"], W=["cvt"])
                        P.op("dve", lambda e: e.scalar_tensor_tensor(out=cvt[:, 1:1 + W_], in0=rawsb[:, 1:1 + W_],
                                                                     scalar=convT[:, l, c, 1:2], in1=cvt[:, 1:1 + W_],
                                                                     op0=ALU.mult, op1=ALU.add), R=["rawsb", "convT", "cvt"], W=["cvt"])
                        P.op("dve", lambda e: e.scalar_tensor_tensor(out=cvt[:, 1:1 + W_], in0=rawsb[:, 2:2 + W_],
                                                                     scalar=convT[:, l, c, 2:3], in1=cvt[:, 1:1 + W_],
                                                                     op0=ALU.mult, op1=ALU.add), R=["rawsb", "convT", "cvt"], W=["cvt"])
                        P.op("dve", lambda e: e.tensor_copy(out=prelast[:, c:c + 1], in_=cvt[:, H_LAT + NL - 1:H_LAT + NL]),
                             R=["cvt"], W=["prelast"])
                        P.op("act", lambda e: e.activation(out=mqkT[:, c, 0:NL], in_=cvt[:, H_LAT:H_LAT + NL], func=AF.Silu),
                             R=["cvt"], W=MQ[0:4])
                        P.op("act", lambda e: e.activation(out=mqkT[:, c, NL:T], in_=cvt[:, H_CTX:H_CTX + NCX], func=AF.Silu),
                             R=["cvt"], W=MQ[4:5])
                    P.barrier()
                if cfg.get('stop_at') == 'P1':
                    return True
                import os as _os
                _parts = _os.environ.get("P2_PARTS", "ABCDEF")
                with ExitStack() as st:
                    wmv = al(st, [128, KC, 272], BF16, "wmv")
                    bgbc = al(st, [128, 16], F32, "bgbc")
                    note_mem()
                    if "A" in _parts:
                        P.dma("pool", out=wmv[:], in_=wv[:, :, C_MV:C_MV + 272], W=["wmv"])
                    if "B" in _parts:
                        P.dma("sp", out=bgbc[:], in_=bgate_d[l].partition_broadcast(128), W=["bgbc"])
                    if "C" in _parts:
                        P.op("pool", lambda e: e.memset(mv_aug[:, :, :, 64:65], 1.0), W=["mv_ones"])
                    for c in range(int(_os.environ.get("P2_NCH", NCH))):
                        tc0, hc0 = chunk_cols(c)
                        pa = ps[c % 2]
                        if "D" in _parts:
                            for kc in range(KC):
                                P.op("pe", lambda e: e.matmul(pa[:, 0:272], lhsT=hxT[:, kc, hc0:hc0 + 128], rhs=wmv[:, kc, :],
                                                              start=(kc == 0), stop=(kc == KC - 1)),
                                     R=["wmv"] + HXALL, W=[("ps", c % 2)])
                        if "E" in _parts:
                            P.op("dve", lambda e: e.tensor_copy(out=mv_aug[:, c, :, 0:64], in_=pa[:, 0:256].rearrange("p (k d) -> p k d", d=64)),
                                 R=[("ps", c % 2), "mv_ones"], W=[("mv", c)])
                        if "F" in _parts:
                            P.op("dve", lambda e: e.tensor_tensor(out=li[:, c, :], in0=pa[:, 256:264], in1=bgbc[:, 0:8], op=ALU.add),
                                 R=[("ps", c % 2), "bgbc"], W=["li"])
                            P.op("dve", lambda e: e.tensor_tensor(out=zf[:, c, :], in0=pa[:, 264:272], in1=bgbc[:, 8:16], op=ALU.add),
                                 R=[("ps", c % 2), "bgbc"], W=["zf"])
                    P.barrier()
                if cfg.get('stop_at') == 'P2':
                    return True
                with ExitStack() as st:
                    wk = al(st, [128, KC, 256], BF16, "wk")
                    wvv = al(st, [128, KC, 128], BF16, "wvv")
                    wf = al(st, [128, KC, 256], BF16, "wf")
                    ropeb = al(st, [128, 2, 128], F32, "ropeb")
                    rt = al(st, [128, 2, 128], F32, "rt")
                    ufT = al(st, [128, 2, NL], BF16, "ufT")
                    khb = al(st, [128, 128], BF16, "khb")
                    vhb = al(st, [128, 2, 65], BF16, "vhb")
                    note_mem()
                    P.dma("pool", out=wk[:], in_=wv[:, :, C_K:C_K + 256], W=["wk"])
                    P.dma("pool", out=wvv[:], in_=wv[:, :, C_V:C_V + 128], W=["wvv"])
                    P.dma("pool", out=wf[:], in_=wv[:, :, C_F:C_F + 256], W=["wf"])
                    P.dma("sp", out=ropeb[:], in_=rope_d[:, :, NL - 128:NL], W=["ropeb"])
                    P.op("pool", lambda e: e.memset(vhb[:, :, 64:65], 1.0), W=["vhb1"])
                    for ti, (xc, hc, n, v) in enumerate(TILES):
                        for ch in range(2):
                            pq_ = ps[ch]
                            for kc in range(KC):
                                P.op("pe", lambda e: e.matmul(pq_[:, 0:n], lhsT=wf[:, kc, ch * 128:(ch + 1) * 128],
                                                              rhs=hxT[:, kc, hc:hc + n], start=(kc == 0), stop=(kc == KC - 1)),
                                     R=["wf"] + HXALL, W=[("ps", ch)])
                            if v == 0:
                                P.op("act", lambda e: e.activation(out=ufT[:, ch, xc:xc + n], in_=pq_[:, 0:n], func=AF.Identity), R=[("ps", ch)], W=["ufT"])
                            else:
                                P.op("act", lambda e: e.activation(out=ufc[:, ch, :], in_=pq_[:, 0:n], func=AF.Identity), R=[("ps", ch)], W=["ufc"])
                    hl = H_LAT + NL - 128
                    for kc in range(KC):
                        P.op("pe", lambda e: e.matmul(ps[2][:, 0:128], lhsT=wk[:, kc, 0:128], rhs=hxT[:, kc, hl:hl + 128],
                                                      start=(kc == 0), stop=(kc == KC - 1)), R=["wk"] + HXALL, W=[("ps", 2)])
                    for kc in range(KC):
                        P.op("pe", lambda e: e.matmul(ps[3][:, 0:128], lhsT=wk[:, kc, 128:256], rhs=hxT[:, kc, hl:hl + 128],
                                                      start=(kc == 0), stop=(kc == KC - 1)), R=["wk"] + HXALL, W=[("ps", 3)])
                    P.op("dve", lambda e: e.tensor_tensor(out=rt[:, 0, :], in0=ps[2][:, 0:128], in1=ropeb[:, 0, :], op=ALU.mult),
                         R=[("ps", 2), "ropeb"], W=["rt0"])
                    P.op("dve", lambda e: e.tensor_tensor(out=rt[:, 1, :], in0=ps[3][:, 0:128], in1=ropeb[:, 1, :], op=ALU.mult),
                         R=[("ps", 3), "ropeb"], W=["rt1"])
                    P.op("dve", lambda e: e.tensor_tensor(out=khb[:], in0=rt[:, 0, :], in1=rt[:, 1, :], op=ALU.add),
                         R=["rt0", "rt1"], W=["khb"])
                    for kc in range(KC):
                        P.op("pe", lambda e: e.matmul(ps[4][:, 0:128], lhsT=hxT[:, kc, hl:hl + 128], rhs=wvv[:, kc, :],
                                                      start=(kc == 0), stop=(kc == KC - 1)), R=["wvv"] + HXALL, W=[("ps", 4)])
                    P.op("dve", lambda e: e.tensor_copy(out=vhb[:, :, 0:64], in_=ps[4][:, 0:128].rearrange("p (k d) -> p k d", d=64)),
                         R=[("ps", 4), "vhb1"], W=["vhb"])
                    P.dma("sp", out=pb_out[l][:, 0:4096].rearrange("p (c t) -> p c t", c=2), in_=ufT[:], R=["ufT"], W=["pb_out"])
                    P.dma("sp", out=pb_out[l][:, 4096:4224], in_=khb[:], R=["khb"], W=["pb_out"])
                    P.dma("sp", out=pb_out[l][:, 4224:4354], in_=vhb[:].rearrange("p k d -> p (k d)"), R=["vhb", "vhb1"], W=["pb_out"])
                    dump(f"mqkT{l}", mqkT[:], [128, 4, T], BF16, R=MQ)
                    P.barrier()
                if cfg.get('stop_at') == 'P3':
                    return True
                P.op("act", lambda e: e.activation(out=gtmp, in_=zf, func=AF.Exp, scale=-1.0), R=["zf"], W=["gtmp"])
                P.op("act", lambda e: e.activation(out=gtmp, in_=gtmp, func=AF.Ln, bias=one_t, scale=1.0), R=["gtmp", "cst"], W=["gtmp"])
                P.op("dve", lambda e: e.tensor_scalar(out=lf, in0=gtmp, scalar1=-1.0, scalar2=None, op0=ALU.mult), R=["gtmp"], W=["lf"])
                P.op("pe", lambda e: e.matmul(ps[0][:, 0:72], lhsT=triA, rhs=lf[:, :, 0:4], start=True, stop=True),
                     R=["lf", "cst"], W=[("ps", 0)])
                P.op("pe", lambda e: e.matmul(ps[0][:, 72:144], lhsT=triB, rhs=lf[:, :, 4:8], start=True, stop=True),
                     R=["lf", "cst"], W=[("ps", 0)])
                P.op("pe", lambda e: e.matmul(ps[1][:, 0:144], lhsT=onesf, rhs=lf, start=True, stop=True),
                     R=["lf", "cst"], W=[("ps", 1)])
                P.op("dve", lambda e: e.tensor_copy(out=bcum[:, :, 0:4], in_=ps[0][:, 0:72].rearrange("p (c g) -> p c g", g=4)),
                     R=[("ps", 0)], W=["bcum"])
                P.op("dve", lambda e: e.tensor_copy(out=bcum[:, :, 4:8], in_=ps[0][:, 72:144].rearrange("p (c g) -> p c g", g=4)),
                     R=[("ps", 0)], W=["bcum"])
                P.op("dve", lambda e: e.tensor_copy(out=btot, in_=ps[1][:, 0:144].rearrange("p (c g) -> p c g", g=8)),
                     R=[("ps", 1)], W=["btot"])
                P.op("act", lambda e: e.activation(out=eq, in_=bcum, func=AF.Exp), R=["bcum"], W=["eq"])
                P.op("dve", lambda e: e.tensor_tensor(out=gtmp, in0=li, in1=bcum, op=ALU.subtract), R=["li", "bcum"], W=["gtmp"])
                P.op("act", lambda e: e.activation(out=ek, in_=gtmp, func=AF.Exp), R=["gtmp"], W=["ek"])
                P.op("dve", lambda e: e.tensor_scalar(out=ek, in0=ek, scalar1=0.125, scalar2=None, op0=ALU.mult), R=["ek"], W=["ek"])
                P.op("act", lambda e: e.activation(out=edec, in_=btot, func=AF.Exp), R=["btot"], W=["edec"])
                P.op("dve", lambda e: e.tensor_tensor(out=ev, in0=ek, in1=edec, op=ALU.mult), R=["ek", "edec"], W=["ev"])
                dump(f"gat{l}", gat[:], [128, 10, NCH, 8], F32, R=["eq", "ek", "ev", "edec", "li", "lf", "bcum", "btot"])

                if cfg.get('stop_at') == 'gates':
                    P.barrier()
                    return True
                with ExitStack() as st:
                    hsum = al(st, [128, NCH, 256], F32, "hsum")
                    Dg = al(st, [128, 4, 128], F32, "Dg")
                    Wt = al(st, [128, 4, 128], F32, "Wt")
                    AT = al(st, [128, 4, 128], BF16, "AT")
                    Nsb = al(st, [128, 4, 65], F32, "Nsb")
                    tmpI = al(st, [128, 4, 65], F32, "tmpI")
                    k2 = al(st, [128, 4, 64], BF16, "k2")
                    Sst = al(st, [128, 3, 2, 130], F32, "Sst")
                    Sbb = al(st, [128, 3, 2, 130], BF16, "Sbb")
                    dmx = al(st, [128, 8], F32, "dmx")
                    htmp = al(st, [128, 4, 64], F32, "htmp")
                    evz = al(st, [128, 4], F32, "evz")
                    vrt = al(st, [128, 4, 65], F32, "vrt")
                    gfs = al(st, [128, 2, PFW], F32, "gfs")
                    vrb = al(st, [128, 2, 260], F32, "vrb")
                    sm = al(st, [128, 16], F32, "sm")
                    wmo = al(st, [128, KC, 256], BF16, "wmo")
                    sgo = al(st, [128, 256], F32, "sgo")
                    note_mem()
                    P.dma("pool", out=wmo[:], in_=wv[:, :, C_MO:C_MO + 256], W=["wmo"])
                    P.op("pool", lambda e: e.memset(Sst[:], 0.0), W=["S0", "S1", "S2"])
                    P.op("pool", lambda e: e.memset(Sbb[:], 0.0), W=["Sb0", "Sb1", "Sb2"])

                    def state_update(si, c, g0, evs, so=None):
                        so = si if so is None else so
                        tc0, _ = chunk_cols(c)
                        for p in range(2):
                            P.op("pe", lambda e: e.transpose(out=psb[:, p * 128:(p + 1) * 128], in_=mqkT[:, 2 + p, tc0:tc0 + 128],
                                                             identity=identb), R=MQ + ["cst"], W=["psb"])
                        P.op("dve", lambda e: e.tensor_tensor(out=k2[:], in0=psb[:, 0:256].rearrange("p (h d) -> p h d", d=64),
                                                              in1=evs.unsqueeze(2).to_broadcast([128, 4, 64]), op=ALU.mult),
                             R=["psb", "ev", "evz"], W=["k2"])
                        for p in range(2):
                            P.op("pe", lambda e: e.matmul(ps[4][:, p * 130:(p + 1) * 130],
                                                          lhsT=k2[:, 2 * p:2 * p + 2, :].rearrange("p h d -> p (h d)"),
                                                          rhs=mv_aug[:, c, 2 * p:2 * p + 2, :].rearrange("p h d -> p (h d)"),
                                                          start=True, stop=True), R=["k2", ("mv", c), "mv_ones"], W=[("ps", 4)])
                        for h in range(4):
                            p = h // 2
                            r0 = (h % 2) * 64
                            c0 = (h % 2) * 65
                            P.op("dve", lambda e: e.scalar_tensor_tensor(
                                out=Sst[r0:r0 + 64, so, p, c0:c0 + 65], in0=Sst[r0:r0 + 64, si, p, c0:c0 + 65],
                                scalar=edec[r0:r0 + 64, c, g0 + h:g0 + h + 1],
                                in1=ps[4][r0:r0 + 64, p * 130 + c0:p * 130 + c0 + 65],
                                op0=ALU.mult, op1=ALU.add), R=[f"S{si}", "edec", ("ps", 4)], W=[f"S{so}"])
                        P.op("act", lambda e: e.activation(out=Sbb[:, so, :, :], in_=Sst[:, so, :, :], func=AF.Identity), R=[f"S{so}"], W=[f"Sb{so}"])

                    def scan_chunk(si, c, dirn):
                        g0 = 0 if dirn == 0 else 4
                        mask = triA if dirn == 0 else triB
                        tc0, _ = chunk_cols(c)
                        P.op("dve", lambda e: e.tensor_tensor(out=Dg[:], in0=identf.unsqueeze(1).to_broadcast([128, 4, 128]),
                                                              in1=eq[:, c, g0:g0 + 4].unsqueeze(2).to_broadcast([128, 4, 128]),
                                                              op=ALU.mult), R=["cst", "eq"], W=["Dg"])
                        P.op("pe", lambda e: e.matmul(ps[0][:, 0:512], lhsT=onesf, rhs=Dg[:].rearrange("p h t -> p (h t)"),
                                                      start=True, stop=True), R=["Dg", "cst"], W=[("ps", 0)])
                        P.op("dve", lambda e: e.tensor_tensor(out=Wt[:], in0=ps[0][:, 0:512].rearrange("p (h t) -> p h t", h=4),
                                                              in1=mask.unsqueeze(1).to_broadcast([128, 4, 128]), op=ALU.mult),
                             R=[("ps", 0), "cst"], W=["Wt"])
                        for h in range(4):
                            b0 = (h % 2) * 64
                            sbk = 1 if h % 2 == 0 else 5
                            P.op("pe", lambda e: e.matmul(ps[sbk][:, (h // 2) * 128:(h // 2 + 1) * 128],
                                                          lhsT=mqkT[b0:b0 + 64, 2 + h // 2, tc0:tc0 + 128],
                                                          rhs=mqkT[b0:b0 + 64, h // 2, tc0:tc0 + 128], start=True, stop=True),
                                 R=MQ, W=[("ps", sbk)])
                        for h in range(4):
                            sbk = 1 if h % 2 == 0 else 5
                            P.op("dve", lambda e: e.scalar_tensor_tensor(out=AT[:, h, :], in0=ps[sbk][:, (h // 2) * 128:(h // 2 + 1) * 128],
                                                                         scalar=ek[:, c, g0 + h:g0 + h + 1], in1=Wt[:, h, :],
                                                                         op0=ALU.mult, op1=ALU.mult),
                                 R=[("ps", sbk), "ek", "Wt"], W=["AT"])
                        for h in range(4):
                            P.op("pe", lambda e: e.matmul(ps[2][:, h * 65:(h + 1) * 65], lhsT=AT[:, h, :], rhs=mv_aug[:, c, h, :],
                                                          start=True, stop=True), R=["AT", ("mv", c), "mv_ones"], W=[("ps", 2)])
                        for p in range(2):
                            P.op("pe", lambda e: e.matmul(ps[3][:, p * 130:(p + 1) * 130], lhsT=mqkT[:, p, tc0:tc0 + 128],
                                                          rhs=Sbb[:, si, p, :], start=True, stop=True), R=MQ + [f"Sb{si}"], W=[("ps", 3)])
                        P.op("act", lambda e: e.activation(out=Nsb[:], in_=ps[2][:, 0:260].rearrange("p (h d) -> p h d", d=65), func=AF.Identity),
                             R=[("ps", 2)], W=["Nsb"])
                        P.op("dve", lambda e: e.tensor_tensor(out=tmpI[:], in0=ps[3][:, 0:260].rearrange("p (h d) -> p h d", d=65),
                                                              in1=eq[:, c, g0:g0 + 4].unsqueeze(2).to_broadcast([128, 4, 65]), op=ALU.mult),
                             R=[("ps", 3), "eq"], W=["tmpI"])
                        P.op("dve", lambda e: e.tensor_tensor(out=Nsb[:], in0=Nsb[:], in1=tmpI[:], op=ALU.add), R=["Nsb", "tmpI"], W=["Nsb"])
                        P.op("dve", lambda e: e.scalar_tensor_tensor(out=dmx[:, 0:4], in0=Nsb[:, :, 64], scalar=-1.0, in1=Nsb[:, :, 64],
                                                                     op0=ALU.mult, op1=ALU.max), R=["Nsb"], W=["dmx"])
                        P.op("dve", lambda e: e.tensor_scalar(out=dmx[:, 0:4], in0=dmx[:, 0:4], scalar1=1.0, scalar2=None, op0=ALU.max),
                             R=["dmx"], W=["dmx"])
                        P.op("dve", lambda e: e.reciprocal(out=dmx[:, 4:8], in_=dmx[:, 0:4]), R=["dmx"], W=["dmx"])
                        hv = hsum[:, c, :].rearrange("p (h d) -> p h d", d=64)
                        if dirn == 0:
                            P.op("dve", lambda e: e.tensor_tensor(out=hv, in0=Nsb[:, :, 0:64],
                                                                  in1=dmx[:, 4:8].unsqueeze(2).to_broadcast([128, 4, 64]), op=ALU.mult),
                                 R=["Nsb", "dmx"], W=[("hsum", c)])
                        else:
                            P.op("dve", lambda e: e.tensor_tensor(out=htmp[:], in0=Nsb[:, :, 0:64],
                                                                  in1=dmx[:, 4:8].unsqueeze(2).to_broadcast([128, 4, 64]), op=ALU.mult),
                                 R=["Nsb", "dmx"], W=["htmp"])
                            P.op("pool", lambda e: e.tensor_tensor(out=hv, in0=hv, in1=htmp[:], op=ALU.add),
                                 R=["htmp", ("hsum", c)], W=[("hsum", c)])
                        state_update(si, c, g0, ev[:, c, g0:g0 + 4])

                    for c in (16, 17) + tuple(range(15)):
                        scan_chunk(0, c, 0)
                        if cfg.get('stop_at') == 'scan1':
                            P.barrier()
                            return True
                    if cfg.get('stop_at') == 'scanA':
                        P.barrier()
                        return True
                    P.op("dve", lambda e: e.tensor_scalar(out=evz[:], in0=ev[:, 15, 0:4], scalar1=notlast, scalar2=None, op0=ALU.mult),
                         R=["ev", "cst"], W=["evz"])
                    state_update(0, 15, 0, evz[:], so=2)
                    P.op("dve", lambda e: e.tensor_tensor(out=vrt[:], in0=mv_aug[:, 15, :, :],
                                                          in1=ev[:, 15, 0:4].unsqueeze(2).to_broadcast([128, 4, 65]), op=ALU.mult),
                         R=[("mv", 15), "mv_ones", "ev"], W=["vrt"])
                    P.dma("sp", out=pf_out[l][:, 0:4], in_=cb_own[:], R=["cb_own"], W=["pf_out"])
                    P.dma("sp", out=pf_out[l][:, 4:264], in_=Sst[:, 2, :, :].rearrange("p a b -> p (a b)"), R=["S2"], W=["pf_out"])
                    P.dma("sp", out=pf_out[l][:, 264:266], in_=prelast[:, 2:4], R=["prelast"], W=["pf_out"])
                    P.dma("sp", out=pf_out[l][:, 266:526], in_=vrt[:].rearrange("p a b -> p (a b)"), R=["vrt"], W=["pf_out"])
                    if exch != "cc":
                        out_keys.extend(["pb_out", "pf_out"])
                    if not last:
                        for c in (17, 16):
                            scan_chunk(1, c, 1)
                    if stop_after_payload:
                        P.barrier()
                        return True

                    P.barrier()
                    if exch == "cc":
                        RG = [[0, 1], [2, 3], [4, 5], [6, 7]]
                        P.cc(lambda e: e.collective_compute("AllGather", ALU.bypass, replica_groups=RG,
                                                            ins=[pb_out[l].opt()], outs=[gb_raw[l].opt()]),
                             R=["pb_out"], W=["gb"])
                        P.cc(lambda e: e.collective_compute("AllGather", ALU.bypass, replica_groups=RG,
                                                            ins=[pf_out[l].opt()], outs=[gf_raw[l].opt()]),
                             R=["pf_out"], W=["gf"])
                    P.dma("sp", out=gfs[:], in_=gf_in[l].rearrange("s p w -> p s w"), R=["gf"], W=["gfs"])
                    for s_ in range(2):
                        P.dma("sp", out=vrb[:, s_, :], in_=gf_in[l][s_, 127, 266:526].partition_broadcast(128), R=["gf"], W=["vrb"])

                    def nbsel(out_ap, a0, a1, Rk, Wk):
                        P.op("dve", lambda e: e.tensor_scalar(out=out_ap, in0=a0, scalar1=selv[:, 0:1], scalar2=None, op0=ALU.mult),
                             R=Rk + ["cst"], W=Wk)
                        P.op("dve", lambda e: e.scalar_tensor_tensor(out=out_ap, in0=a1, scalar=selv[:, 1:2], in1=out_ap,
                                                                     op0=ALU.mult, op1=ALU.add), R=Rk + Wk + ["cst"], W=Wk)
                    nbsel(sm[:, 0:4], gfs[:, 0, 0:4], gfs[:, 1, 0:4], ["gfs"], ["sm"])
                    nbsel(sm[:, 4:6], gfs[:, 0, 264:266], gfs[:, 1, 264:266], ["gfs"], ["sm"])
                    nbsel(Sst[:, 1, :, :].rearrange("p a b -> p (a b)"), gfs[:, 0, 4:264], gfs[:, 1, 4:264], ["gfs", "Sb1"], ["S1"])
                    nbsel(vrt[:].rearrange("p a b -> p (a b)"), vrb[:, 0, :], vrb[:, 1, :], ["vrb"], ["vrt"])
                    P.op("dve", lambda e: e.tensor_tensor(out=sm[:, 8:12], in0=prelast[:], in1=sm[:, 0:4], op=ALU.add),
                         R=["prelast", "sm"], W=["sm"])
                    P.op("act", lambda e: e.activation(out=sm[:, 12:16], in_=sm[:, 8:12], func=AF.Silu), R=["sm"], W=["sm"])
                    P.op("dve", lambda e: e.tensor_copy(out=mqkT[:, :, NL - 1:NL], in_=sm[:, 12:16].unsqueeze(2)), R=["sm"], W=MQ)
                    P.op("dve", lambda e: e.tensor_tensor(out=sm[:, 6:8], in0=sm[:, 4:6], in1=cb_own[:, 2:4], op=ALU.add),
                         R=["sm", "cb_own"], W=["sm"])
                    P.op("act", lambda e: e.activation(out=sm[:, 6:8], in_=sm[:, 6:8], func=AF.Silu), R=["sm"], W=["sm"])
                    for h in range(4):
                        p = h // 2
                        r0 = (h % 2) * 64
                        c0 = (h % 2) * 65
                        P.op("dve", lambda e: e.scalar_tensor_tensor(
                            out=Sst[r0:r0 + 64, 1, p, c0:c0 + 65], in0=vrt[r0:r0 + 64, h, :], scalar=sm[r0:r0 + 64, 6 + p:7 + p],
                            in1=Sst[r0:r0 + 64, 1, p, c0:c0 + 65], op0=ALU.mult, op1=ALU.add), R=["vrt", "sm", "S1"], W=["S1"])
                    P.op("act", lambda e: e.activation(out=Sbb[:, 1, :, :], in_=Sst[:, 1, :, :], func=AF.Identity), R=["S1"], W=["Sb1"])
                    scan_chunk(0, 15, 0)
                    for c in range(15, -1, -1):
                        scan_chunk(1, c, 1)
                    dump(f"hsum{l}", hsum[:], [128, NCH, 256], F32, R=[("hsum", c) for c in range(NCH)])
                    for c in range(NCH):
                        if last and c >= 16:
                            continue
                        tc0, hc0 = chunk_cols(c)
                        pa = ps[5 + c % 2]
                        for kc in range(KC):
                            P.op("pe", lambda e: e.matmul(pa[:, 0:256], lhsT=hxT[:, kc, hc0:hc0 + 128], rhs=wmo[:, kc, :],
                                                          start=(kc == 0), stop=(kc == KC - 1)), R=["wmo"] + HXALL, W=[("ps", 5 + c % 2)])
                        P.op("act", lambda e: e.activation(out=sgo[:], in_=pa[:, 0:256], func=AF.Sigmoid), R=[("ps", 5 + c % 2)], W=["sgo"])
                        P.op("dve", lambda e: e.tensor_tensor(out=AT[:, 0:2, :].rearrange("p a b -> p (a b)"), in0=hsum[:, c, :],
                                                              in1=sgo[:], op=ALU.mult), R=[("hsum", c), "sgo"], W=["AT"])
                        for p in range(2):
                            P.op("pe", lambda e: e.transpose(out=psb[:, p * 128:(p + 1) * 128], in_=AT[:, p, :], identity=identb),
                                 R=["AT", "cst"], W=["psb"])
                        P.op("act", lambda e: e.activation(out=mlT[:, :, tc0:tc0 + 128], in_=psb[:, 0:256].rearrange("p (a t) -> p a t", a=2), func=AF.Identity),
                             R=["psb"], W=["mlT"])
                    P.barrier()
            dump(f"mlT{l}", mlT[:], [128, 2, T], BF16, R=["mlT"])

            with ExitStack() as st_at:
                attT = al(st_at, [128, 4, T], BF16, "attT")
                with ExitStack() as st:
                    watt = al(st, [128, KC, 1408], BF16, "watt")
                    kT = al(st, [128, T], BF16, "kT")
                    V_aug = al(st, [128, NCH, 2, 65], BF16, "Vaug")
                    ropek = al(st, [128, 2, 512], F32, "ropek")
                    ropeq = [al(st, [128, 2, 128], F32, "ropeq") for _ in range(2)]
                    t12 = al(st, [128, 2, 4, 128], F32, "t12")
                    rt = t12[:].rearrange("p a c t -> p a (c t)")
                    qblk = al(st, [128, 2, 4, 128], BF16, "qblk")
                    PT = [al(st, [128, 512], BF16, "PT") for _ in range(7)]
                    att_tok = al(st, [128, 8, 64], BF16, "att_tok")
                    khalo = al(st, [128, 2, 128], BF16, "khalo")
                    vhalo = al(st, [128, 2, 130], BF16, "vhalo")
                    esink = al(st, [128, 8], F32, "esink")
                    dn = al(st, [128, 8], F32, "dn")
                    note_mem()
                    P.dma("pool", out=watt[:], in_=wv[:, :, C_Q:C_Q + 1408], W=["watt"])
                    P.dma("sp", out=khalo[:], in_=gb_in[l][:, :, 4096:4224].rearrange("s p w -> p s w"), R=["gb"], W=["khalo"])
                    P.dma("sp", out=vhalo[:], in_=gb_in[l][:, :, 4224:4354].rearrange("s p w -> p s w"), R=["gb"], W=["vhalo"])
                    P.dma("sp", out=esink[:], in_=sink_d[l].partition_broadcast(128), W=["esink"])
                    P.op("act", lambda e: e.activation(out=esink[:], in_=esink[:], func=AF.Exp), R=["esink"], W=["esink"])
                    P.op("pool", lambda e: e.memset(V_aug[:, :, :, 64:65], 1.0), W=["v_ones"])
                    P.op("pool", lambda e: e.memset(qblk[:], 0.0), W=["qblk"])
                    WQ, WQP, WK, WKP, WV = 0, 512, 1024, 1152, 1280
                    for ti, (xc, hc, n, v) in enumerate(TILES):
                        for kc in range(KC):
                            P.op("pe", lambda e: e.matmul(ps[0][:, 0:n], lhsT=watt[:, kc, WK:WK + 128], rhs=hxT[:, kc, hc:hc + n],
                                                          start=(kc == 0), stop=(kc == KC - 1)), R=["watt"] + HXALL, W=[("ps", 0)])
                        if v == 0:
                            P.dma("sp", out=ropek[:, :, 0:n], in_=rope_d[:, :, xc:xc + n], W=["ropek"])
                            for kc in range(KC):
                                P.op("pe", lambda e: e.matmul(ps[1][:, 0:n], lhsT=watt[:, kc, WKP:WKP + 128], rhs=hxT[:, kc, hc:hc + n],
                                                              start=(kc == 0), stop=(kc == KC - 1)), R=["watt"] + HXALL, W=[("ps", 1)])
                            P.op("dve", lambda e: e.tensor_tensor(out=rt[:, 0, 0:n], in0=ps[0][:, 0:n], in1=ropek[:, 0, 0:n], op=ALU.mult),
                                 R=[("ps", 0), "ropek"], W=["t1"])
                            P.op("dve", lambda e: e.tensor_tensor(out=rt[:, 1, 0:n], in0=ps[1][:, 0:n], in1=ropek[:, 1, 0:n], op=ALU.mult),
                                 R=[("ps", 1), "ropek"], W=["t2"])
                            P.op("pool", lambda e: e.tensor_tensor(out=kT[:, xc:xc + n], in0=rt[:, 0, 0:n], in1=rt[:, 1, 0:n], op=ALU.add),
                                 R=["t1", "t2"], W=["kT"])
                        else:
                            P.op("act", lambda e: e.activation(out=kT[:, xc:xc + n], in_=ps[0][:, 0:n], func=AF.Identity), R=[("ps", 0)], W=["kT"])
                    for c in range(NCH):
                        tc0, hc0 = chunk_cols(c)
                        pa = ps[2 + c % 2]
                        for kc in range(KC):
                            P.op("pe", lambda e: e.matmul(pa[:, 0:128], lhsT=hxT[:, kc, hc0:hc0 + 128], rhs=watt[:, kc, WV:WV + 128],
                                                          start=(kc == 0), stop=(kc == KC - 1)), R=["watt"] + HXALL, W=[("ps", 2 + c % 2)])
                        P.op("dve", lambda e: e.tensor_copy(out=V_aug[:, c, :, 0:64], in_=pa[:, 0:128].rearrange("p (k d) -> p k d", d=64)),
                             R=[("ps", 2 + c % 2), "v_ones"], W=["V"])
                    dump(f"kT{l}", kT[:], [128, T], BF16, R=["kT"])
                    blocks = list(range(16)) + ([] if last else [16, 17])
                    for bi, i in enumerate(blocks):
                        is_ctx = i >= 16
                        tc0, hc0 = chunk_cols(i)
                        for c in range(4):
                            for kc in range(KC):
                                P.op("pe", lambda e: e.matmul(ps[0][:, c * 128:(c + 1) * 128], lhsT=watt[:, kc, WQ + c * 128:WQ + (c + 1) * 128],
                                                              rhs=hxT[:, kc, hc0:hc0 + 128], start=(kc == 0), stop=(kc == KC - 1)),
                                     R=["watt"] + HXALL, W=[("ps", 0)])
                        if not is_ctx:
                            rq = ropeq[bi % 2]
                            P.dma("sp", out=rq[:], in_=rope_d[:, :, tc0:tc0 + 128], W=[("ropeq", bi % 2)])
                            for c in range(4):
                                for kc in range(KC):
                                    P.op("pe", lambda e: e.matmul(ps[1][:, c * 128:(c + 1) * 128],
                                                                  lhsT=watt[:, kc, WQP + c * 128:WQP + (c + 1) * 128],
                                                                  rhs=hxT[:, kc, hc0:hc0 + 128], start=(kc == 0), stop=(kc == KC - 1)),
                                         R=["watt"] + HXALL, W=[("ps", 1)])
                            P.op("dve", lambda e: e.tensor_tensor(out=t12[:, 0, :, :], in0=ps[0][:, 0:512].rearrange("p (c t) -> p c t", c=4),
                                                                  in1=rq[:, 0, :].unsqueeze(1).to_broadcast([128, 4, 128]), op=ALU.mult),
                                 R=[("ps", 0), ("ropeq", bi % 2)], W=["t1"])
                            P.op("dve", lambda e: e.tensor_tensor(out=t12[:, 1, :, :], in0=ps[1][:, 0:512].rearrange("p (c t) -> p c t", c=4),
                                                                  in1=rq[:, 1, :].unsqueeze(1).to_broadcast([128, 4, 128]), op=ALU.mult),
                                 R=[("ps", 1), ("ropeq", bi % 2)], W=["t2"])
                            for kv_ in range(2):
                                P.op("pool", lambda e: e.tensor_tensor(out=qblk[kv_ * 64:(kv_ + 1) * 64, kv_, :, :],
                                                                       in0=t12[kv_ * 64:(kv_ + 1) * 64, 0, :, :],
                                                                       in1=t12[kv_ * 64:(kv_ + 1) * 64, 1, :, :], op=ALU.add),
                                     R=["t1", "t2"], W=["qblk"])
                        else:
                            for kv_ in range(2):
                                P.op("dve", lambda e: e.tensor_copy(out=qblk[kv_ * 64:(kv_ + 1) * 64, kv_, :, :],
                                                                    in_=ps[0][kv_ * 64:(kv_ + 1) * 64, 0:512].rearrange("p (c t) -> p c t", c=4)),
                                     R=[("ps", 0)], W=["qblk"])
                        for kvh in range(2):
                            b0 = kvh * 64
                            klist = []
                            if not is_ctx:
                                if i > 0:
                                    klist.append((kT[:, 128 * (i - 1):128 * i], m_prev, V_aug[:, i - 1, kvh, :]))
                                klist.append((kT[:, 128 * i:128 * (i + 1)], None, V_aug[:, i, kvh, :]))
                                if i < 15:
                                    klist.append((kT[:, 128 * (i + 1):128 * (i + 2)], m_next, V_aug[:, i + 1, kvh, :]))
                                else:
                                    for s_ in range(2):
                                        klist.append((khalo[:, s_, :], mh[s_], vhalo[:, s_, kvh * 65:(kvh + 1) * 65]))
                            for cc in (16, 17):
                                tcc, _ = chunk_cols(cc)
                                klist.append((kT[:, tcc:tcc + 128], None, V_aug[:, cc, kvh, :]))
                            ob = ps[4 + kvh]
                            nk = len(klist)
                            for idx, (kap, mask, vap) in enumerate(klist):
                                spb = 2 + idx % 2
                                P.op("pe", lambda e: e.matmul(ps[spb][:, 0:512], lhsT=kap, rhs=qblk[:, kvh, :, :], start=True, stop=True),
                                     R=["kT", "khalo", "qblk"], W=[("ps", spb)])
                                ptb = PT[idx]
                                P.op("act", lambda e: e.activation(out=ptb[:], in_=ps[spb][:, 0:512], func=AF.Exp, scale=0.125),
                                     R=[("ps", spb)], W=[("PT", idx)])
                                if mask is not None:
                                    P.op("pool", lambda e: e.tensor_tensor(out=ptb[:].rearrange("p (c t) -> p c t", c=4),
                                                                           in0=ptb[:].rearrange("p (c t) -> p c t", c=4),
                                                                           in1=mask.unsqueeze(1).to_broadcast([128, 4, 128]), op=ALU.mult),
                                         R=[("PT", idx), "cst"], W=[("PT", idx)])
                            for j in range(4):
                                for idx, (kap, mask, vap) in enumerate(klist):
                                    P.op("pe", lambda e: e.matmul(ob[:, j * 65:(j + 1) * 65], lhsT=PT[idx][:, j * 128:(j + 1) * 128], rhs=vap,
                                                                  start=(idx == 0), stop=(idx == nk - 1)),
                                         R=[("PT", idx), "V", "vhalo", "v_ones"], W=[("ps", 4 + kvh)])
                            ov = ob[:, 0:260].rearrange("p (h d) -> p h d", d=65)
                            P.op("dve", lambda e: e.tensor_tensor(out=dn[:, 4 * kvh:4 * kvh + 4], in0=ov[:, :, 64],
                                                                  in1=esink[:, 4 * kvh:4 * kvh + 4], op=ALU.add),
                                 R=[("ps", 4 + kvh), "esink"], W=[("dn", kvh)])
                            P.op("dve", lambda e: e.reciprocal(out=dn[:, 4 * kvh:4 * kvh + 4], in_=dn[:, 4 * kvh:4 * kvh + 4]),
                                 R=[("dn", kvh)], W=[("dn", kvh)])
                            P.op("dve", lambda e: e.tensor_tensor(out=att_tok[:, 4 * kvh:4 * kvh + 4, :], in0=ov[:, :, 0:64],
                                                                  in1=dn[:, 4 * kvh:4 * kvh + 4].unsqueeze(2).to_broadcast([128, 4, 64]),
                                                                  op=ALU.mult), R=[("ps", 4 + kvh), ("dn", kvh)], W=["att_tok"])
                        for cc in range(4):
                            P.op("pe", lambda e: e.transpose(out=psb[:, cc * 128:(cc + 1) * 128],
                                                             in_=att_tok[:, 2 * cc:2 * cc + 2, :].rearrange("p h d -> p (h d)"),
                                                             identity=identb), R=["att_tok", "cst"], W=["psb"])
                        P.op("act", lambda e: e.activation(out=attT[:, :, tc0:tc0 + 128], in_=psb[:, 0:512].rearrange("p (c t) -> p c t", c=4), func=AF.Identity),
                             R=["psb"], W=["attT"])
                    P.barrier()
                dump(f"attT{l}", attT[:], [128, 4, T], BF16, R=["attT"])

                with ExitStack() as st_f:
                    yfT = al(st_f, [128, 2, T], BF16, "yfT")
                    with ExitStack() as st:
                        ufg = al(st, [128, 2, 2, NL], BF16, "ufg")
                        wbd = al(st, [128, 2, 128], F32, "wbd")
                        Gbd = al(st, [128, 2, 256], BF16, "Gbd")
                        PQb = [al(st, [128, 512], BF16, "PQb") for _ in range(2)]
                        dftb = [al(st, [128, 2, 2, 512], BF16, "dftb") for _ in range(3)]
                        dftc = al(st, [128, 2, 2, 256], BF16, "dftc")
                        note_mem()
                        for s_ in range(2):
                            P.dma("sp", out=ufg[:, s_, :, :], in_=gb_in[l][s_, :, 0:4096].rearrange("p (c t) -> p c t", c=2), R=["gb"], W=["ufg"])
                        P.op("pool", lambda e: e.memset(wbd[:], 0.0), W=["wbd"])
                        for g in range(4):
                            r0 = (g % 2) * 64
                            P.dma("sp", out=wbd[r0:r0 + 64, g // 2, r0:r0 + 64], in_=wfour_d[l, g], R=["wbd"], W=[("wbdd", g)])
                        for ch in range(2):
                            for k_ in range(2):
                                P.op("pe", lambda e: e.matmul(ps[6][:, ch * 256 + k_ * 128:ch * 256 + (k_ + 1) * 128],
                                                              lhsT=ccs[:, k_ * 128:(k_ + 1) * 128], rhs=wbd[:, ch, :], start=True, stop=True),
                                     R=["cst", "wbd"] + [("wbdd", g) for g in range(4)], W=[("ps", 6)])
                        P.op("act", lambda e: e.activation(out=Gbd[:], in_=ps[6][:, 0:512].rearrange("p (c w) -> p c w", c=2), func=AF.Identity), R=[("ps", 6)], W=["Gbd"])
                        for pass_ in range(2):
                            for nchunk in range(32):
                                slot, j = divmod(nchunk, 16)
                                pqp = ps[4 + nchunk % 2]
                                for ch in range(2):
                                    P.op("pe", lambda e: e.matmul(pqp[:, ch * 256:(ch + 1) * 256], lhsT=ufg[:, slot, ch, j * 128:(j + 1) * 128],
                                                                  rhs=Gbd[:, ch, :], start=True, stop=True), R=["ufg", "Gbd"], W=[("ps", 4 + nchunk % 2)])
                                pqb = PQb[nchunk % 2]
                                if nchunk % 2 == 0:
                                    P.op("act", lambda e: e.activation(out=pqb[:], in_=pqp[:, 0:512], func=AF.Identity), R=[("ps", 4 + nchunk % 2)], W=[("PQb", nchunk % 2)])
                                else:
                                    P.op("dve", lambda e: e.tensor_copy(out=pqb[:], in_=pqp[:, 0:512]), R=[("ps", 4 + nchunk % 2)], W=[("PQb", nchunk % 2)])
                                db = dftb[nchunk % 3]
                                P.dma("sp", out=db[:], in_=dftl_d[pass_, nchunk], W=[("dftb", nchunk % 3)])
                                for ktl in range(2):
                                    for ec in range(2):
                                        acc = ps[ktl * 2 + ec]
                                        P.op("pe", lambda e: e.matmul(acc[:, 0:512], lhsT=pqb[:, ec * 256:ec * 256 + 128], rhs=db[:, ktl, 0, :],
                                                                      start=(nchunk == 0), stop=False),
                                             R=[("PQb", nchunk % 2), ("dftb", nchunk % 3)], W=[("ps", ktl * 2 + ec)])
                                        P.op("pe", lambda e: e.matmul(acc[:, 0:512], lhsT=pqb[:, ec * 256 + 128:ec * 256 + 256], rhs=db[:, ktl, 1, :],
                                                                      start=False, stop=(nchunk == 31)),
                                             R=[("PQb", nchunk % 2), ("dftb", nchunk % 3)], W=[("ps", ktl * 2 + ec)])
                            for ktl in range(2):
                                for ec in range(2):
                                    k0 = (pass_ * 2 + ktl) * 512
                                    P.op("act", lambda e: e.activation(out=yfT[:, ec, k0:k0 + 512], in_=ps[ktl * 2 + ec][:, 0:512], func=AF.Identity),
                                         R=[("ps", ktl * 2 + ec)], W=["yfT"])
                        if not last:
                            P.dma("sp", out=dftc[:], in_=dftc_d, W=["dftc"])
                            for nck in range(2):
                                pqp = ps[4 + nck]
                                for ch in range(2):
                                    P.op("pe", lambda e: e.matmul(pqp[:, ch * 256:(ch + 1) * 256], lhsT=ufc[:, ch, nck * 128:(nck + 1) * 128],
                                                                  rhs=Gbd[:, ch, :], start=True, stop=True), R=["ufc", "Gbd"], W=[("ps", 4 + nck)])
                                P.op("act", lambda e: e.activation(out=PQb[nck][:], in_=pqp[:, 0:512], func=AF.Identity), R=[("ps", 4 + nck)], W=[("PQb", nck)])
                            for ec in range(2):
                                for nck in range(2):
                                    P.op("pe", lambda e: e.matmul(ps[ec][:, 0:256], lhsT=PQb[nck][:, ec * 256:ec * 256 + 128], rhs=dftc[:, nck, 0, :],
                                                                  start=(nck == 0), stop=False), R=[("PQb", nck), "dftc"], W=[("ps", ec)])
                                    P.op("pe", lambda e: e.matmul(ps[ec][:, 0:256], lhsT=PQb[nck][:, ec * 256 + 128:ec * 256 + 256], rhs=dftc[:, nck, 1, :],
                                                                  start=False, stop=(nck == 1)), R=[("PQb", nck), "dftc"], W=[("ps", ec)])
                                P.op("act", lambda e: e.activation(out=yfT[:, ec, NL:T], in_=ps[ec][:, 0:256], func=AF.Identity), R=[("ps", ec)], W=["yfT"])
                        P.barrier()
                    dump(f"yfT{l}", yfT[:], [128, 2, T], BF16, R=["yfT"])

                    with ExitStack() as st:
                        wo = al(st, [128, KC, D], BF16, "wo")
                        note_mem()
                        P.dma("pool", out=wo[:], in_=wout_d[l].rearrange("(rc p) n -> p rc n", p=128), W=["wo"])
                        srcs = [yfT[:, 0, :], yfT[:, 1, :], attT[:, 0, :], attT[:, 1, :], attT[:, 2, :], attT[:, 3, :], mlT[:, 0, :], mlT[:, 1, :]]
                        it = 0
                        for oc in range(KC):
                            for ti in (range(5) if not last else range(4)):
                                xc, hc, n, v = TILES[ti]
                                pbk = it % 4
                                it += 1
                                for rc in range(KC):
                                    P.op("pe", lambda e: e.matmul(ps[pbk][:, 0:n], lhsT=wo[:, rc, oc * 128:(oc + 1) * 128], rhs=srcs[rc][:, xc:xc + n],
                                                                  start=(rc == 0), stop=(rc == KC - 1)),
                                         R=["wo", "yfT", "attT", "mlT"], W=[("ps", pbk)])
                                P.op("dve", lambda e: e.scalar_tensor_tensor(out=xT[:, oc, xc:xc + n], in0=ps[pbk][:, 0:n],
                                                                             scalar=g_ap(1, v, oc), in1=xT[:, oc, xc:xc + n],
                                                                             op0=ALU.mult, op1=ALU.add),
                                     R=[("ps", pbk), ("x", ti), "G_t"], W=[("x", ti)])
                        P.barrier()
        return False

    nlayers = cfg.get("layers", DEPTH)
    stop_payload = cfg.get("stop_payload", False)
    stopped = False
    for l in range(nlayers):
        last = (l == DEPTH - 1)
        mods_phase(l)
        dump(f"modT{l}", modT[:], [128, 72, 2], R=["modT"])
        if not cfg.get('skip_ffn'):
            ffn_phase(l, 0, 0, range(5))
        dump(f"x_ffn0_{l}", xT[:], [128, KC, T], R=[("x", i) for i in range(5)])
        if cfg.get("mixer", True):
            stopped = mixer_phase(l, stop_payload and l == nlayers - 1)
            if stopped:
                break
            dump(f"x_mix_{l}", xT[:], [128, KC, T], R=[("x", i) for i in range(5)])
        ffn_phase(l, 1, 2, range(5) if not last else range(4))
        dump(f"x_ffn1_{l}", xT[:], [128, KC, T], R=[("x", i) for i in range(5)])
    if nlayers == DEPTH and not stopped:
        final_phase()
    else:
        P.finish(out_keys)
        P.barrier()
    print("min sbuf bytes remaining:", minrem[0], "instr counts:", P.cnt)
    return nc


def _fm(a):
    t = a.shape[0]
    return np.ascontiguousarray(a.reshape(t, KC, 128).transpose(2, 1, 0))


def host_shared(inp):
    sh = {}
    sh["w_ada"] = np.ascontiguousarray(inp["w_ada"], dtype=np.float32)
    sh["b_adaT"] = np.ascontiguousarray(inp["b_ada"].reshape(DEPTH, 72, 128).transpose(2, 0, 1))
    sh["norm_gT"] = np.ascontiguousarray(inp["norm_g"].reshape(DEPTH, 3, KC, 128).transpose(3, 0, 1, 2))
    sh["g_finT"] = np.ascontiguousarray(inp["g_final"].reshape(KC, 128).T)
    w1 = inp["w_ffn_in"]
    g = w1[..., :DFF].reshape(DEPTH, 2, KC, 128, NJ, 128)
    u = w1[..., DFF:].reshape(DEPTH, 2, KC, 128, NJ, 128)
    gu = np.concatenate([g, u], axis=-1)
    sh["W1r"] = np.ascontiguousarray(gu.transpose(0, 1, 4, 3, 2, 5))
    sh["W2"] = np.ascontiguousarray(inp["w_ffn_out"], dtype=np.float32)
    return sh


def host_core(inp, b, h):
    flip = (h == 1)
    xs = inp["x"][b, h * NL:(h + 1) * NL]
    cx = inp["ctx"][b]
    if flip:
        xs = xs[::-1]
        cx = cx[::-1]
    m = {}
    m["xT0"] = _fm(np.concatenate([xs, cx], axis=0))
    m["cT"] = np.ascontiguousarray(np.stack([inp["c"][b], inp["c_ctx"]], axis=-1).reshape(KC, 128, 2).transpose(1, 0, 2))
    return m


_BF = ml_dtypes.bfloat16
_PERM = np.concatenate([np.arange(16, 32), np.arange(0, 16), np.arange(48, 64), np.arange(32, 48)])
_SIGN = np.concatenate([-np.ones(16), np.ones(16), -np.ones(16), np.ones(16)]).astype(np.float32)


def host_consts(parity):
    c = {}
    r = np.arange(128)
    cf = np.zeros((128, 1024), np.float32)
    cf[:, 0:128] = np.eye(128)
    cf[:, 128:256] = (r[:, None] <= r[None, :])
    cf[:, 256:384] = (r[:, None] >= r[None, :])
    cf[:, 384:512] = 1.0
    k64 = np.arange(64)
    ang = 2 * np.pi * ((k64[:, None] * k64[None, :]) % 64) / 64.0
    Cc = np.cos(ang) / 8.0
    Sc = np.sin(ang) / 8.0
    cf[0:64, 512:576] = Cc
    cf[64:128, 576:640] = Cc
    cf[0:64, 640:704] = Sc
    cf[64:128, 704:768] = Sc
    cf[:, 768] = 0.0 if parity == 0 else 1.0
    cf[:, 769] = 1.0 if parity == 0 else 0.0
    cf[:, 770] = 1.0
    cf[127, 770] = 0.0
    c["cst_f"] = cf
    cb = np.zeros((128, 640), np.float32)
    cb[:, 0:128] = np.eye(128)
    cb[:, 128:256] = (r[:, None] >= r[None, :])
    cb[:, 256:384] = (r[:, None] <= r[None, :])
    hm = ((r[:, None] + r[None, :]) >= 127).astype(np.float32)
    if parity == 0:
        cb[:, 512:640] = hm
    else:
        cb[:, 384:512] = hm
    c["cst_b"] = cb.astype(_BF)
    t = np.arange(NL)
    g = t if parity == 0 else (2 * NL - 1 - t)
    row = (g // 64).astype(np.float32)
    col = (g % 64).astype(np.float32)
    inv = (np.float32(10000.0) ** (-(np.arange(16, dtype=np.float32) / np.float32(16)))).astype(np.float32)
    ar = row[:, None] * inv[None, :]
    ac = col[:, None] * inv[None, :]
    angr = np.concatenate([ar, ar, ac, ac], axis=-1).astype(np.float32)
    cosT = np.cos(angr).T.astype(np.float32)
    sinT = (np.sin(angr).T * _SIGN[:, None]).astype(np.float32)
    rope = np.zeros((128, 2, NL), np.float32)
    rope[0:64, 0] = cosT
    rope[64:128, 0] = cosT
    rope[0:64, 1] = sinT
    rope[64:128, 1] = sinT
    c["rope"] = rope
    N = 2 * NL
    tabc = np.cos(2 * np.pi * np.arange(N) / N) / 64.0
    tabs = -np.sin(2 * np.pi * np.arange(N) / N) / 64.0
    j = np.arange(NL)
    nglob = np.concatenate([j, N - 1 - j])
    kl = np.arange(NL)
    kglob = kl if parity == 0 else (N - 1 - kl)
    idx = (nglob[:, None].astype(np.int64) * kglob[None, :].astype(np.int64)) % N
    dl = np.empty((2, 32, 128, 2, 2, 512), _BF)
    ic = tabc[idx].reshape(32, 128, 2, 2, 512)
    isn = tabs[idx].reshape(32, 128, 2, 2, 512)
    dl[:, :, :, :, 0, :] = ic.transpose(2, 0, 1, 3, 4).astype(_BF)
    dl[:, :, :, :, 1, :] = isn.transpose(2, 0, 1, 3, 4).astype(_BF)
    c["dft_lat"] = dl
    nc_ = np.arange(NCX)
    ng = nc_ if parity == 0 else (NCX - 1 - nc_)
    idc = (ng[:, None] * ng[None, :]) % NCX
    tc = np.cos(2 * np.pi * np.arange(NCX) / NCX) / 16.0
    ts = -np.sin(2 * np.pi * np.arange(NCX) / NCX) / 16.0
    dc = np.empty((128, 2, 2, 256), _BF)
    dc[:, :, 0, :] = tc[idc].reshape(2, 128, 256).transpose(1, 0, 2).astype(_BF)
    dc[:, :, 1, :] = ts[idc].reshape(2, 128, 256).transpose(1, 0, 2).astype(_BF)
    c["dft_ctx"] = dc
    return c


def host_parity(inp, parity):
    m = {}
    w = inp["w_in"]
    L = w.shape[0]
    wp = np.empty((L, D, NCOL), np.float32)
    wp[:, :, C_F:C_F + 256] = w[:, :, 0:256]
    q = w[:, :, 256:768].reshape(L, D, 8, 64)
    qperm = q[:, :, :, _PERM]
    for c in range(4):
        wp[:, :, C_Q + c * 128:C_Q + c * 128 + 64] = q[:, :, c]
        wp[:, :, C_Q + c * 128 + 64:C_Q + (c + 1) * 128] = q[:, :, c + 4]
        wp[:, :, C_QP + c * 128:C_QP + c * 128 + 64] = qperm[:, :, c]
        wp[:, :, C_QP + c * 128 + 64:C_QP + (c + 1) * 128] = qperm[:, :, c + 4]
    k = w[:, :, 768:896].reshape(L, D, 2, 64)
    wp[:, :, C_K:C_K + 128] = k.reshape(L, D, 128)
    wp[:, :, C_KP:C_KP + 128] = k[:, :, :, _PERM].reshape(L, D, 128)
    wp[:, :, C_V:C_V + 128] = w[:, :, 896:1024]
    wp[:, :, C_MV:C_MV + 256] = w[:, :, 1536:1792]
    mi = w[:, :, 2048:2056].reshape(L, D, 2, 4)
    mf = w[:, :, 2056:2064].reshape(L, D, 2, 4)
    if parity == 1:
        mi = mi[:, :, ::-1]
        mf = mf[:, :, ::-1]
    wp[:, :, C_G:C_G + 8] = mi.reshape(L, D, 8)
    wp[:, :, C_G + 8:C_G + 16] = mf.reshape(L, D, 8)
    wp[:, :, C_MO:C_MO + 256] = w[:, :, 1792:2048]
    wp[:, :, C_MQK:C_MQK + 512] = w[:, :, 1024:1536]
    m["w_in_p"] = wp
    cq = inp["conv_qk"]
    if parity == 1:
        cq = cq[:, ::-1]
    m["convT"] = np.ascontiguousarray(cq.reshape(L, 3, 4, 128).transpose(3, 0, 2, 1))
    bi = inp["b_gate_i"]
    bf = inp["b_gate_f"]
    if parity == 1:
        bi = bi[:, ::-1]
        bf = bf[:, ::-1]
    m["bgate"] = np.ascontiguousarray(np.concatenate([bi.reshape(L, 8), bf.reshape(L, 8)], axis=1))
    return m


def host_all(inp):
    inp = {k: np.asarray(v, dtype=np.float32) for k, v in inp.items()}
    sh = host_shared(inp)
    sh["sink"] = np.ascontiguousarray(inp["attn_sink"])
    sh["w_fourier"] = np.ascontiguousarray(inp["w_fourier"])
    sh["w_out"] = np.ascontiguousarray(inp["w_out"])
    par = []
    for p in range(2):
        d = dict(host_consts(p))
        d.update(host_parity(inp, p))
        par.append(d)
    maps = []
    for b in range(4):
        for h in range(2):
            m = dict(sh)
            m.update(par[h])
            m.update(host_core(inp, b, h))
            for l in range(DEPTH):
                m[f"gb_in{l}"] = np.zeros((2, 128, PBW), _BF)
                m[f"gf_in{l}"] = np.zeros((2, 128, PFW), np.float32)
            maps.append(m)
    return maps


def _exchange(maps, res, l):
    for b in range(4):
        pb = np.stack([res.results[2 * b][f"pb_out{l}"], res.results[2 * b + 1][f"pb_out{l}"]])
        pf = np.stack([res.results[2 * b][f"pf_out{l}"], res.results[2 * b + 1][f"pf_out{l}"]])
        for h in range(2):
            maps[2 * b + h][f"gb_in{l}"] = pb
            maps[2 * b + h][f"gf_in{l}"] = pf


_NC_CACHE = {}


def _get_nc(key, cfg):
    if key not in _NC_CACHE:
        _NC_CACHE[key] = build(cfg)
    return _NC_CACHE[key]


def kernel(**inputs):
    maps = host_all(inputs)
    cores = list(range(8))
    for m in maps:
        for l in range(DEPTH):
            m.pop(f"gb_in{l}", None)
            m.pop(f"gf_in{l}", None)
    res = run_bass_kernel_spmd(build(dict(layers=2, exch="cc")), maps, core_ids=cores)
    out = np.empty((4, 2 * NL, D), np.float32)
    for b in range(4):
        for h in range(2):
            o = res.results[2 * b + h]["outT"]
            tok = o.transpose(2, 1, 0).reshape(NL, D)
            if h == 1:
                tok = tok[::-1]
            out[b, h * NL:(h + 1) * NL] = tok
    return out
```

```python
import numpy as np
import ml_dtypes
import concourse.bass as bass
import concourse.mybir as mybir
from concourse.bass_utils import run_bass_kernel_spmd

F32 = mybir.dt.float32
BF16 = mybir.dt.bfloat16
AF = mybir.ActivationFunctionType
ALU = mybir.AluOpType
AX = mybir.AxisListType

D = 1024
KC = 8
NL = 2048
NCX = 256
T = NL + NCX
DFF = 2816
NJ = 22
DEPTH = 2
EPS = 1e-6
H_LAT = 2
H_HALO = H_LAT + NL
H_CTX = H_HALO + 2
TP = H_CTX + NCX + 2
TILES = [(0, H_LAT, 512, 0), (512, H_LAT + 512, 512, 0), (1024, H_LAT + 1024, 512, 0),
         (1536, H_LAT + 1536, 512, 0), (2048, H_CTX, 256, 1)]
NCH = 18
QUARTERS = [(0, 6), (6, 12), (12, 17), (17, 22)]

C_F = 0
C_Q = 256
C_QP = 768
C_K = 1280
C_KP = 1408
C_V = 1536
C_T2 = 1920
C_END = 2192
C_MQK = 2192
NCOL = 2704


class Prog:
    def __init__(self, nc):
        self.nc = nc
        self.eng = {"pe": nc.tensor, "dve": nc.vector, "act": nc.scalar, "pool": nc.gpsimd, "sp": nc.sync}
        self.sem = {k: nc.alloc_semaphore("prog_" + k) for k in self.eng}
        self.cnt = {k: 0 for k in self.eng}
        self.NS = 8
        self.dsem = {q: [nc.alloc_semaphore(f"dma_{q}_{i}") for i in range(self.NS)] for q in ("sp", "pool", "act")}
        self.dcnt = {q: [0] * self.NS for q in self.dsem}
        self.dnext = {q: 0 for q in self.dsem}
        self.waited = {k: {} for k in self.eng}
        self.last_w = {}
        self.readers = {}
        self.semobj = {}
        for k, s in self.sem.items():
            self.semobj[("e", k)] = s
        for q, lst in self.dsem.items():
            for i, s in enumerate(lst):
                self.semobj[("d", q, i)] = s

    def _wait(self, eng, sid, val):
        if val <= 0:
            return
        if self.waited[eng].get(sid, 0) >= val:
            return
        self.eng[eng].wait_ge(self.semobj[sid], val)
        self.waited[eng][sid] = val

    def _deps(self, eng, R, W, skip_self=False):
        for k in R:
            lw = self.last_w.get(k)
            if lw is not None:
                if not (skip_self and lw[0] == ("e", eng)):
                    self._wait(eng, lw[0], lw[1])
        for k in W:
            lw = self.last_w.get(k)
            if lw is not None:
                if not (skip_self and lw[0] == ("e", eng)):
                    self._wait(eng, lw[0], lw[1])
            for sid, val in self.readers.get(k, {}).items():
                if not (skip_self and sid == ("e", eng)):
                    self._wait(eng, sid, val)

    def _record(self, sid, val, R, W):
        for k in W:
            self.last_w[k] = (sid, val)
            self.readers[k] = {}
        for k in R:
            d = self.readers.setdefault(k, {})
            if d.get(sid, 0) < val:
                d[sid] = val

    def op(self, eng, fn, R=(), W=()):
        self._deps(eng, R, W, skip_self=(eng == "pe"))
        inst = fn(self.eng[eng])
        self.cnt[eng] += 1
        inst.then_inc(self.sem[eng], 1)
        self._record(("e", eng), self.cnt[eng], R, W)
        return inst

    def dma(self, q, out, in_, R=(), W=(), **kw):
        self._deps(q, R, W)
        i = self.dnext[q]
        self.dnext[q] = (i + 1) % self.NS
        sid = ("d", q, i)
        self._wait(q, sid, self.dcnt[q][i])
        self.eng[q].dma_start(out=out, in_=in_, **kw).then_inc(self.dsem[q][i], 16)
        self.dcnt[q][i] += 16
        self._record(sid, self.dcnt[q][i], R, W)

    def cc(self, fn, R=(), W=()):
        self._deps("pool", R, W)
        sem = self.nc.alloc_semaphore(f"cc_{len(self.semobj)}")
        sid = ("c", len(self.semobj))
        self.semobj[sid] = sem
        fn(self.eng["pool"]).then_inc(sem)
        self._record(sid, 1, R, W)

    def barrier(self):
        for e in self.eng:
            for k in self.eng:
                if k != e:
                    self._wait(e, ("e", k), self.cnt[k])
            for q in self.dsem:
                for i in range(self.NS):
                    self._wait(e, ("d", q, i), self.dcnt[q][i])

    def finish(self, keys):
        for k in keys:
            lw = self.last_w.get(k)
            if lw is not None:
                self._wait("sp", lw[0], lw[1])


C_F = 0
C_Q = 256
C_QP = 768
C_K = 1280
C_KP = 1408
C_V = 1536
C_MV = 1664
C_G = 1920
C_MO = 1936
C_MQK = 2192
NCOL = 2704
PBW = 4354
PFW = 526


def build(cfg):
    from contextlib import ExitStack
    nc = bass.Bass("TRN2", target_bir_lowering=False)
    P = Prog(nc)
    dbg = cfg.get("dbg", set())
    uid = [0]

    def din(name, shape, dt=F32):
        return nc.dram_tensor(name, list(shape), dt, kind="ExternalInput").ap()

    def dout(name, shape, dt=F32):
        return nc.dram_tensor(name, list(shape), dt, kind="ExternalOutput").ap()

    def sb(name, shape, dt=F32):
        return nc.alloc_sbuf_tensor(name, list(shape), dt)

    def al(st, shape, dt=F32, name="t"):
        uid[0] += 1
        return st.enter_context(nc.sbuf_tensor(f"{name}_{uid[0]}", list(shape), dt))

    out_keys = []
    minrem = [1 << 30]

    def note_mem():
        minrem[0] = min(minrem[0], nc.sbuf_bytes_remaining)

    def dump(name, src_ap, shape, dt=F32, R=()):
        if name not in dbg:
            return
        d = dout("dbg_" + name, shape, dt)
        P.dma("sp", out=d, in_=src_ap, R=list(R), W=[("dbgout", name)])
        out_keys.append(("dbgout", name))

    x_in = din("xT0", [128, KC, T])
    cT_d = din("cT", [128, KC, 2])
    w_ada = din("w_ada", [DEPTH, D, 9 * D])
    b_adaT = din("b_adaT", [128, DEPTH, 72])
    norm_gT = din("norm_gT", [128, DEPTH, 3, KC])
    g_finT = din("g_finT", [128, KC])
    W1r = din("W1r", [DEPTH, 2, NJ, 128, KC, 256])
    W2 = din("W2", [DEPTH, 2, DFF, D])
    outT = dout("outT", [128, KC, NL])
    w_in_d = din("w_in_p", [DEPTH, D, NCOL])
    convT_d = din("convT", [128, DEPTH, 4, 3])
    bgate_d = din("bgate", [DEPTH, 16])
    sink_d = din("sink", [DEPTH, 8])
    wfour_d = din("w_fourier", [DEPTH, 4, 64, 64])
    wout_d = din("w_out", [DEPTH, D, D])
    cst_f = din("cst_f", [128, 1024])
    cst_b = din("cst_b", [128, 640], BF16)
    rope_d = din("rope", [128, 2, NL])
    dftl_d = din("dft_lat", [2, 32, 128, 2, 2, 512], BF16)
    dftc_d = din("dft_ctx", [128, 2, 2, 256], BF16)
    exch = cfg.get("exch", "input")
    if exch == "cc":
        pb_out = [nc.dram_tensor(f"pb_int{l}", [128, PBW], BF16).ap() for l in range(DEPTH)]
        pf_out = [nc.dram_tensor(f"pf_int{l}", [128, PFW], F32).ap() for l in range(DEPTH)]
        gb_raw = [nc.dram_tensor(f"gb_int{l}", [256, PBW], BF16).ap() for l in range(DEPTH)]
        gf_raw = [nc.dram_tensor(f"gf_int{l}", [256, PFW], F32).ap() for l in range(DEPTH)]
        gb_in = [a.rearrange("(s p) w -> s p w", s=2) for a in gb_raw]
        gf_in = [a.rearrange("(s p) w -> s p w", s=2) for a in gf_raw]
    else:
        pb_out = [dout(f"pb_out{l}", [128, PBW], BF16) for l in range(DEPTH)]
        pf_out = [dout(f"pf_out{l}", [128, PFW], F32) for l in range(DEPTH)]
        gb_in = [din(f"gb_in{l}", [2, 128, PBW], BF16) for l in range(DEPTH)]
        gf_in = [din(f"gf_in{l}", [2, 128, PFW], F32) for l in range(DEPTH)]

    xT = sb("xT", [128, KC, T])
    hxT = sb("hxT", [128, KC, TP], BF16)
    ones_bf = sb("ones_bf", [128, 128], BF16)
    eps_t = sb("eps_t", [128, 1])
    scT = sb("scT", [128, KC, 2])
    badaT = sb("badaT", [128, DEPTH, 72])
    normg = sb("normg", [128, DEPTH, 3, KC])
    gfin = sb("gfin", [128, KC])
    convT = sb("convT_sb", [128, DEPTH, 4, 3])
    modT = sb("modT", [128, 72, 2])
    A_t = sb("A_t", [128, 3, KC, 2])
    G_t = sb("G_t", [128, 3, KC, 2])
    cf = sb("cst_f_sb", [128, 1024])
    cb_ = sb("cst_b_sb", [128, 640], BF16)
    ps = [nc.alloc_psum_tensor(f"ps{i}", [128, 512], F32) for i in range(7)]
    psb = nc.alloc_psum_tensor("psb", [128, 1024], BF16)

    P.op("pool", lambda e: e.memset(ones_bf[:], 1.0), W=["ones_bf"])
    P.op("pool", lambda e: e.memset(eps_t[:], EPS), W=["eps"])
    P.op("pool", lambda e: e.memset(hxT[:], 0.0), W=[("hx", i) for i in range(5)] + ["hxpad"])
    P.dma("sp", out=xT[:, :, 0:1024], in_=x_in[:, :, 0:1024], W=[("x", 0), ("x", 1)])
    P.dma("sp", out=xT[:, :, 1024:T], in_=x_in[:, :, 1024:T], W=[("x", 2), ("x", 3), ("x", 4)])
    P.dma("sp", out=scT[:], in_=cT_d, W=["scT"])
    P.dma("sp", out=badaT[:], in_=b_adaT, W=["bada"])
    P.dma("sp", out=normg[:], in_=norm_gT, W=["normg"])
    P.dma("sp", out=gfin[:], in_=g_finT, W=["gfin"])
    P.dma("sp", out=convT[:], in_=convT_d, W=["convT"])
    P.dma("sp", out=cf[:], in_=cst_f, W=["cst"])
    P.dma("sp", out=cb_[:], in_=cst_b, W=["cst"])
    P.op("act", lambda e: e.activation(out=scT[:], in_=scT[:], func=AF.Silu), R=["scT"], W=["scT"])
    identf = cf[:, 0:128]
    triA = cf[:, 128:256]
    triB = cf[:, 256:384]
    onesf = cf[:, 384:512]
    ccs = cf[:, 512:768]
    selv = cf[:, 768:770]
    notlast = cf[:, 770:771]
    one_t = cf[:, 384:385]
    identb = cb_[:, 0:128]
    m_prev = cb_[:, 128:256]
    m_next = cb_[:, 256:384]
    mh = [cb_[:, 384:512], cb_[:, 512:640]]
    HXALL = [("hx", i) for i in range(5)] + ["hxpad"]

    def a_ap(sub, v, kc):
        return A_t[:, sub, kc, v:v + 1]

    def sh_ap(sub, v, kc):
        return modT[:, (3 * sub) * 8 + kc, v:v + 1]

    def g_ap(sub, v, kc):
        return G_t[:, sub, kc, v:v + 1]

    def chunk_cols(c):
        if c < 16:
            return 128 * c, H_LAT + 128 * c
        return NL + 128 * (c - 16), H_CTX + 128 * (c - 16)

    def mods_phase(l):
        with ExitStack() as st:
            wada_buf = [al(st, [128, KC, 512], F32, "wada") for _ in range(2)]
            note_mem()
            for cg in range(18):
                wb = wada_buf[cg % 2]
                P.dma("sp", out=wb[:], in_=w_ada[l, :, cg * 512:(cg + 1) * 512].rearrange("(kc p) n -> p kc n", p=128),
                      W=[("wada", cg % 2)])
                for cc in range(4):
                    col = cg * 4 + cc
                    for k in range(KC):
                        P.op("pe", lambda e: e.matmul(ps[5][:, col * 2:col * 2 + 2], lhsT=wb[:, k, cc * 128:(cc + 1) * 128],
                                                      rhs=scT[:, k, :], start=(k == 0), stop=(k == KC - 1)),
                             R=[("wada", cg % 2), "scT"], W=[("ps", 5)])
            P.op("dve", lambda e: e.tensor_tensor(out=modT[:], in0=ps[5][:, 0:144].rearrange("p (c v) -> p c v", v=2),
                                                  in1=badaT[:, l, :].unsqueeze(2).to_broadcast([128, 72, 2]), op=ALU.add),
                 R=[("ps", 5), "bada"], W=["modT"])
            for sub in range(3):
                P.op("dve", lambda e: e.scalar_tensor_tensor(
                    out=A_t[:, sub, :, :], in0=modT[:, (3 * sub + 1) * 8:(3 * sub + 2) * 8, :], scalar=1.0,
                    in1=normg[:, l, sub, :].unsqueeze(2).to_broadcast([128, KC, 2]), op0=ALU.add, op1=ALU.mult),
                    R=["modT", "normg"], W=["A_t"])
                P.op("dve", lambda e: e.tensor_scalar(out=G_t[:, sub, :, :], in0=modT[:, (3 * sub + 2) * 8:(3 * sub + 3) * 8, :],
                                                      scalar1=(0.5 if sub != 1 else 1.0), scalar2=None, op0=ALU.mult),
                     R=["modT"], W=["G_t"])
            P.barrier()

    def rstd_tile(ti, NSC):
        sq, rs, rstd = NSC["sq"], NSC["rs"], NSC["rstd"]
        xc, hc, n, v = TILES[ti]
        P.op("act", lambda e: e.activation(out=sq[:, :, 0:n], in_=xT[:, :, xc:xc + n], func=AF.Square),
             R=[("x", ti)], W=["sq"])
        for kc in range(KC):
            P.op("pe", lambda e: e.matmul(ps[6][:, 0:n], lhsT=ones_bf[:], rhs=sq[:, kc, 0:n], start=(kc == 0),
                                          stop=(kc == KC - 1)), R=["sq", "ones_bf"], W=[("ps", 6)])
        P.op("act", lambda e: e.activation(out=rs[:, 0:n], in_=ps[6][:, 0:n], func=AF.Sqrt, bias=eps_t[:], scale=1.0 / D),
             R=[("ps", 6), "eps"], W=["rs"])
        P.op("dve", lambda e: e.reciprocal(out=rstd[:, 0:n], in_=rs[:, 0:n]), R=["rs"], W=["rstd"])

    def norm_scratch(st):
        return {"sq": al(st, [128, KC, 512], BF16, "sq"), "rs": al(st, [128, 512], F32, "rs"),
                "rstd": al(st, [128, 512], F32, "rstd"), "tmp": [al(st, [128, 512], F32, "tmp") for _ in range(2)]}

    def norm_phase(sub):
        with ExitStack() as st:
            NSC = norm_scratch(st)
            note_mem()
            tmp = NSC["tmp"]
            rstd = NSC["rstd"]
            for ti, (xc, hc, n, v) in enumerate(TILES):
                rstd_tile(ti, NSC)
                for kc in range(KC):
                    tb = tmp[kc % 2]
                    P.op("dve", lambda e: e.scalar_tensor_tensor(out=tb[:, 0:n], in0=xT[:, kc, xc:xc + n], scalar=a_ap(sub, v, kc),
                                                                 in1=rstd[:, 0:n], op0=ALU.mult, op1=ALU.mult),
                         R=[("x", ti), "rstd", "A_t"], W=[("tmp", kc % 2)])
                    P.op("act", lambda e: e.activation(out=hxT[:, kc, hc:hc + n], in_=tb[:, 0:n], func=AF.Identity,
                                                       bias=sh_ap(sub, v, kc), scale=1.0),
                         R=[("tmp", kc % 2), "modT"], W=[("hx", ti)])
            P.barrier()

    def ffn_phase(l, f, sub, tiles):
        norm_phase(sub)
        with ExitStack() as st:
            actT = al(st, [128, 6, T], BF16, "actT")
            w1buf = [al(st, [128, KC, 256], BF16, "w1b") for _ in range(3)]
            w2buf = [al(st, [128, 6, D], BF16, "w2b") for _ in range(2)]
            sg = [al(st, [128, 512], F32, "sg") for _ in range(2)]
            note_mem()
            for qi, (j0, j1) in enumerate(QUARTERS):
                nj = j1 - j0
                w2b = w2buf[qi % 2]
                P.dma("pool", out=w2b[:, 0:nj, :], in_=W2[l, f, j0 * 128:j1 * 128, :].rearrange("(j p) c -> p j c", p=128),
                      W=[("w2", qi % 2)])
                for j in range(j0, j1):
                    wb = w1buf[j % 3]
                    P.dma("pool", out=wb[:], in_=W1r[l, f, j], W=[("w1", j % 3)])
                    for ti in tiles:
                        xc, hc, n, v = TILES[ti]
                        gb = ps[ti % 2]
                        ub = ps[2 + ti % 2]
                        for kc in range(KC):
                            P.op("pe", lambda e: e.matmul(gb[:, 0:n], lhsT=wb[:, kc, 0:128], rhs=hxT[:, kc, hc:hc + n],
                                                          start=(kc == 0), stop=(kc == KC - 1)),
                                 R=[("w1", j % 3), ("hx", ti)], W=[("ps", ti % 2)])
                        for kc in range(KC):
                            P.op("pe", lambda e: e.matmul(ub[:, 0:n], lhsT=wb[:, kc, 128:256], rhs=hxT[:, kc, hc:hc + n],
                                                          start=(kc == 0), stop=(kc == KC - 1)),
                                 R=[("w1", j % 3), ("hx", ti)], W=[("ps", 2 + ti % 2)])
                        sgb = sg[ti % 2]
                        P.op("act", lambda e: e.activation(out=sgb[:, 0:n], in_=gb[:, 0:n], func=AF.Silu),
                             R=[("ps", ti % 2)], W=[("sg", ti % 2)])
                        P.op("dve", lambda e: e.tensor_tensor(out=actT[:, j - j0, xc:xc + n], in0=sgb[:, 0:n], in1=ub[:, 0:n],
                                                              op=ALU.mult),
                             R=[("sg", ti % 2), ("ps", 2 + ti % 2)], W=[("act", j - j0, ti)])
                it = 0
                for oc in range(KC):
                    for ti in tiles:
                        xc, hc, n, v = TILES[ti]
                        pbk = 4 + it % 2
                        it += 1
                        ob = ps[pbk]
                        for jj in range(nj):
                            P.op("pe", lambda e: e.matmul(ob[:, 0:n], lhsT=w2b[:, jj, oc * 128:(oc + 1) * 128],
                                                          rhs=actT[:, jj, xc:xc + n], start=(jj == 0), stop=(jj == nj - 1)),
                                 R=[("w2", qi % 2), ("act", jj, ti)], W=[("ps", pbk)])
                        P.op("dve", lambda e: e.scalar_tensor_tensor(out=xT[:, oc, xc:xc + n], in0=ob[:, 0:n],
                                                                     scalar=g_ap(sub, v, oc), in1=xT[:, oc, xc:xc + n],
                                                                     op0=ALU.mult, op1=ALU.add),
                             R=[("ps", pbk), ("x", ti), "G_t"], W=[("x", ti)])
            P.barrier()

    def final_phase():
        with ExitStack() as st:
            NSC = norm_scratch(st)
            obuf = [al(st, [128, KC, 512], F32, "ob") for _ in range(2)]
            note_mem()
            for ti in range(4):
                xc, hc, n, v = TILES[ti]
                rstd_tile(ti, NSC)
                ob = obuf[ti % 2]
                for kc in range(KC):
                    P.op("dve", lambda e: e.scalar_tensor_tensor(out=ob[:, kc, :], in0=xT[:, kc, xc:xc + n],
                                                                 scalar=gfin[:, kc:kc + 1], in1=NSC["rstd"][:, 0:n],
                                                                 op0=ALU.mult, op1=ALU.mult),
                         R=[("x", ti), "rstd", "gfin"], W=[("ob", ti % 2)])
                P.dma("sp", out=outT[:, :, xc:xc + n], in_=ob[:], R=[("ob", ti % 2)], W=[("outT", ti)])
                out_keys.append(("outT", ti))
            P.finish(out_keys)
            P.barrier()

    def mixer_phase(l, stop_after_payload=False):
        last = (l == DEPTH - 1)
        MQ = [("mqk", i) for i in range(5)]
        norm_phase(1)
        wv = w_in_d[l].rearrange("(kc p) n -> p kc n", p=128)
        with ExitStack() as st_mix:
            mlT = al(st_mix, [128, 2, T], BF16, "mlT")
            ufc = al(st_mix, [128, 2, NCX], BF16, "ufc")
            cb_own = al(st_mix, [128, 4], F32, "cbown")
            prelast = al(st_mix, [128, 4], F32, "prelast")
            with ExitStack() as st_ml:
                mqkT = al(st_ml, [128, 4, T], BF16, "mqkT")
                mv_aug = al(st_ml, [128, NCH, 4, 65], BF16, "mvaug")
                gat = al(st_ml, [128, 10, NCH, 8], F32, "gat")
                li, zf, lf, bcum, btot, eq, ek, ev, edec, gtmp = [gat[:, i, :, :] for i in range(10)]
                with ExitStack() as st:
                    wraw = al(st, [128, KC, 512], BF16, "wraw")
                    rawsb = al(st, [128, TP], F32, "rawsb")
                    cvt = al(st, [128, TP], F32, "cvt")
                    note_mem()
                    P.dma("pool", out=wraw[:], in_=wv[:, :, C_MQK:C_MQK + 512], W=["wraw"])
                    for c in range(4):
                        P.op("pool", lambda e: e.memset(rawsb[:], 0.0), W=["rawsb"])
                        for ti, (xc, hc, n, v) in enumerate(TILES):
                            pb_ = ps[ti % 2]
                            for kc in range(KC):
                                P.op("pe", lambda e: e.matmul(pb_[:, 0:n], lhsT=wraw[:, kc, c * 128:(c + 1) * 128],
                                                              rhs=hxT[:, kc, hc:hc + n], start=(kc == 0), stop=(kc == KC - 1)),
                                     R=["wraw"] + HXALL, W=[("ps", ti % 2)])
                            P.op("act", lambda e: e.activation(out=rawsb[:, hc:hc + n], in_=pb_[:, 0:n], func=AF.Identity), R=[("ps", ti % 2)], W=["rawsb"])
                        P.op("dve", lambda e: e.tensor_scalar(out=cb_own[:, c:c + 1], in0=rawsb[:, H_LAT + NL - 1:H_LAT + NL],
                                                              scalar1=convT[:, l, c, 0:1], scalar2=None, op0=ALU.mult),
                             R=["rawsb", "convT"], W=["cb_own"])
                        W_ = TP - 2
                        P.op("dve", lambda e: e.tensor_scalar(out=cvt[:, 1:1 + W_], in0=rawsb[:, 0:W_], scalar1=convT[:, l, c, 0:1],
                                                              scalar2=None, op0=ALU.mult), R=["rawsb", "convT"], W=["cvt"])
                        P.op("dve", lambda e: e.scalar_tensor_tensor(out=cvt[:, 1:1 + W_], in0=rawsb[:, 1:1 + W_],
                                                                     scalar=convT[:, l, c, 1:2], in1=cvt[:, 1:1 + W_],
                                                                     op0=ALU.mult, op1=ALU.add), R=["rawsb", "convT", "cvt"], W=["cvt"])
                        P.op("dve", lambda e: e.scalar_tensor_tensor(out=cvt[:, 1:1 + W_], in0=rawsb[:, 2:2 + W_],
                                                                     scalar=convT[:, l, c, 2:3], in1=cvt[:, 1:1 + W_],
                                                                     op0=ALU.mult, op1=ALU.add), R=["rawsb", "convT", "cvt"], W=["cvt"])
                        P.op("dve", lambda e: e.tensor_copy(out=prelast[:, c:c + 1], in_=cvt[:, H_LAT + NL - 1:H_LAT + NL]),
                             R=["cvt"], W=["prelast"])
                        P.op("act", lambda e: e.activation(out=mqkT[:, c, 0:NL], in_=cvt[:, H_LAT:H_LAT + NL], func=AF.Silu),
                             R=["cvt"], W=MQ[0:4])
                        P.op("act", lambda e: e.activation(out=mqkT[:, c, NL:T], in_=cvt[:, H_CTX:H_CTX + NCX], func=AF.Silu),
                             R=["cvt"], W=MQ[4:5])
                    P.barrier()
                if cfg.get('stop_at') == 'P1':
                    return True
                import os as _os
                _parts = _os.environ.get("P2_PARTS", "ABCDEF")
                with ExitStack() as st:
                    wmv = al(st, [128, KC, 272], BF16, "wmv")
                    bgbc = al(st, [128, 16], F32, "bgbc")
                    note_mem()
                    if "A" in _parts:
                        P.dma("pool", out=wmv[:], in_=wv[:, :, C_MV:C_MV + 272], W=["wmv"])
                    if "B" in _parts:
                        P.dma("sp", out=bgbc[:], in_=bgate_d[l].partition_broadcast(128), W=["bgbc"])
                    if "C" in _parts:
                        P.op("pool", lambda e: e.memset(mv_aug[:, :, :, 64:65], 1.0), W=["mv_ones"])
                    for c in range(int(_os.environ.get("P2_NCH", NCH))):
                        tc0, hc0 = chunk_cols(c)
                        pa = ps[c % 2]
                        if "D" in _parts:
                            for kc in range(KC):
                                P.op("pe", lambda e: e.matmul(pa[:, 0:272], lhsT=hxT[:, kc, hc0:hc0 + 128], rhs=wmv[:, kc, :],
                                                              start=(kc == 0), stop=(kc == KC - 1)),
                                     R=["wmv"] + HXALL, W=[("ps", c % 2)])
                        if "E" in _parts:
                            P.op("dve", lambda e: e.tensor_copy(out=mv_aug[:, c, :, 0:64], in_=pa[:, 0:256].rearrange("p (k d) -> p k d", d=64)),
                                 R=[("ps", c % 2), "mv_ones"], W=[("mv", c)])
                        if "F" in _parts:
                            P.op("dve", lambda e: e.tensor_tensor(out=li[:, c, :], in0=pa[:, 256:264], in1=bgbc[:, 0:8], op=ALU.add),
                                 R=[("ps", c % 2), "bgbc"], W=["li"])
                            P.op("dve", lambda e: e.tensor_tensor(out=zf[:, c, :], in0=pa[:, 264:272], in1=bgbc[:, 8:16], op=ALU.add),
                                 R=[("ps", c % 2), "bgbc"], W=["zf"])
                    P.barrier()
                if cfg.get('stop_at') == 'P2':
                    return True
                with ExitStack() as st:
                    wk = al(st, [128, KC, 256], BF16, "wk")
                    wvv = al(st, [128, KC, 128], BF16, "wvv")
                    wf = al(st, [128, KC, 256], BF16, "wf")
                    ropeb = al(st, [128, 2, 128], F32, "ropeb")
                    rt = al(st, [128, 2, 128], F32, "rt")
                    pbs = al(st, [128, PBW], BF16, "pbs")
                    ufT = pbs[:, 0:4096].rearrange("p (c t) -> p c t", c=2)
                    khb = pbs[:, 4096:4224]
                    vhb = pbs[:, 4224:4354].rearrange("p (k d) -> p k d", k=2)
                    note_mem()
                    P.dma("pool", out=wk[:], in_=wv[:, :, C_K:C_K + 256], W=["wk"])
                    P.dma("pool", out=wvv[:], in_=wv[:, :, C_V:C_V + 128], W=["wvv"])
                    P.dma("pool", out=wf[:], in_=wv[:, :, C_F:C_F + 256], W=["wf"])
                    P.dma("sp", out=ropeb[:], in_=rope_d[:, :, NL - 128:NL], W=["ropeb"])
                    P.op("pool", lambda e: e.memset(vhb[:, :, 64:65], 1.0), W=["vhb1"])
                    for ti, (xc, hc, n, v) in enumerate(TILES):
                        for ch in range(2):
                            pq_ = ps[ch]
                            for kc in range(KC):
                                P.op("pe", lambda e: e.matmul(pq_[:, 0:n], lhsT=wf[:, kc, ch * 128:(ch + 1) * 128],
                                                              rhs=hxT[:, kc, hc:hc + n], start=(kc == 0), stop=(kc == KC - 1)),
                                     R=["wf"] + HXALL, W=[("ps", ch)])
                            if v == 0:
                                P.op("act", lambda e: e.activation(out=ufT[:, ch, xc:xc + n], in_=pq_[:, 0:n], func=AF.Identity), R=[("ps", ch)], W=["ufT"])
                            else:
                                P.op("act", lambda e: e.activation(out=ufc[:, ch, :], in_=pq_[:, 0:n], func=AF.Identity), R=[("ps", ch)], W=["ufc"])
                    hl = H_LAT + NL - 128
                    for kc in range(KC):
                        P.op("pe", lambda e: e.matmul(ps[2][:, 0:128], lhsT=wk[:, kc, 0:128], rhs=hxT[:, kc, hl:hl + 128],
                                                      start=(kc == 0), stop=(kc == KC - 1)), R=["wk"] + HXALL, W=[("ps", 2)])
                    for kc in range(KC):
                        P.op("pe", lambda e: e.matmul(ps[3][:, 0:128], lhsT=wk[:, kc, 128:256], rhs=hxT[:, kc, hl:hl + 128],
                                                      start=(kc == 0), stop=(kc == KC - 1)), R=["wk"] + HXALL, W=[("ps", 3)])
                    P.op("dve", lambda e: e.tensor_tensor(out=rt[:, 0, :], in0=ps[2][:, 0:128], in1=ropeb[:, 0, :], op=ALU.mult),
                         R=[("ps", 2), "ropeb"], W=["rt0"])
                    P.op("dve", lambda e: e.tensor_tensor(out=rt[:, 1, :], in0=ps[3][:, 0:128], in1=ropeb[:, 1, :], op=ALU.mult),
                         R=[("ps", 3), "ropeb"], W=["rt1"])
                    P.op("dve", lambda e: e.tensor_tensor(out=khb, in0=rt[:, 0, :], in1=rt[:, 1, :], op=ALU.add),
                         R=["rt0", "rt1"], W=["khb"])
                    for kc in range(KC):
                        P.op("pe", lambda e: e.matmul(ps[4][:, 0:128], lhsT=hxT[:, kc, hl:hl + 128], rhs=wvv[:, kc, :],
                                                      start=(kc == 0), stop=(kc == KC - 1)), R=["wvv"] + HXALL, W=[("ps", 4)])
                    P.op("dve", lambda e: e.tensor_copy(out=vhb[:, :, 0:64], in_=ps[4][:, 0:128].rearrange("p (k d) -> p k d", d=64)),
                         R=[("ps", 4), "vhb1"], W=["vhb"])
                    P.dma("sp", out=pb_out[l], in_=pbs[:], R=["ufT", "khb", "vhb", "vhb1"], W=["pb_out"])
                    dump(f"mqkT{l}", mqkT[:], [128, 4, T], BF16, R=MQ)
                    P.barrier()
                if cfg.get('stop_at') == 'P3':
                    return True
                P.op("act", lambda e: e.activation(out=gtmp, in_=zf, func=AF.Exp, scale=-1.0), R=["zf"], W=["gtmp"])
                P.op("act", lambda e: e.activation(out=gtmp, in_=gtmp, func=AF.Ln, bias=one_t, scale=1.0), R=["gtmp", "cst"], W=["gtmp"])
                P.op("dve", lambda e: e.tensor_scalar(out=lf, in0=gtmp, scalar1=-1.0, scalar2=None, op0=ALU.mult), R=["gtmp"], W=["lf"])
                P.op("pe", lambda e: e.matmul(ps[0][:, 0:72], lhsT=triA, rhs=lf[:, :, 0:4], start=True, stop=True),
                     R=["lf", "cst"], W=[("ps", 0)])
                P.op("pe", lambda e: e.matmul(ps[0][:, 72:144], lhsT=triB, rhs=lf[:, :, 4:8], start=True, stop=True),
                     R=["lf", "cst"], W=[("ps", 0)])
                P.op("pe", lambda e: e.matmul(ps[1][:, 0:144], lhsT=onesf, rhs=lf, start=True, stop=True),
                     R=["lf", "cst"], W=[("ps", 1)])
                P.op("dve", lambda e: e.tensor_copy(out=bcum[:, :, 0:4], in_=ps[0][:, 0:72].rearrange("p (c g) -> p c g", g=4)),
                     R=[("ps", 0)], W=["bcum"])
                P.op("dve", lambda e: e.tensor_copy(out=bcum[:, :, 4:8], in_=ps[0][:, 72:144].rearrange("p (c g) -> p c g", g=4)),
                     R=[("ps", 0)], W=["bcum"])
                P.op("dve", lambda e: e.tensor_copy(out=btot, in_=ps[1][:, 0:144].rearrange("p (c g) -> p c g", g=8)),
                     R=[("ps", 1)], W=["btot"])
                P.op("act", lambda e: e.activation(out=eq, in_=bcum, func=AF.Exp), R=["bcum"], W=["eq"])
                P.op("dve", lambda e: e.tensor_tensor(out=gtmp, in0=li, in1=bcum, op=ALU.subtract), R=["li", "bcum"], W=["gtmp"])
                P.op("act", lambda e: e.activation(out=ek, in_=gtmp, func=AF.Exp), R=["gtmp"], W=["ek"])
                P.op("dve", lambda e: e.tensor_scalar(out=ek, in0=ek, scalar1=0.125, scalar2=None, op0=ALU.mult), R=["ek"], W=["ek"])
                P.op("act", lambda e: e.activation(out=edec, in_=btot, func=AF.Exp), R=["btot"], W=["edec"])
                P.op("dve", lambda e: e.tensor_tensor(out=ev, in0=ek, in1=edec, op=ALU.mult), R=["ek", "edec"], W=["ev"])
                dump(f"gat{l}", gat[:], [128, 10, NCH, 8], F32, R=["eq", "ek", "ev", "edec", "li", "lf", "bcum", "btot"])

                if cfg.get('stop_at') == 'gates':
                    P.barrier()
                    return True
                with ExitStack() as st:
                    hsum = al(st, [128, NCH, 256], F32, "hsum")
                    Dg = al(st, [128, 4, 128], F32, "Dg")
                    Wt = al(st, [128, 4, 128], F32, "Wt")
                    AT = al(st, [128, 4, 128], BF16, "AT")
                    Nsb = al(st, [128, 4, 65], F32, "Nsb")
                    tmpI = al(st, [128, 4, 65], F32, "tmpI")
                    k2 = al(st, [128, 4, 64], BF16, "k2")
                    Sst = al(st, [128, 3, 2, 130], F32, "Sst")
                    Sbb = al(st, [128, 3, 2, 130], BF16, "Sbb")
                    dmx = al(st, [128, 8], F32, "dmx")
                    htmp = al(st, [128, 4, 64], F32, "htmp")
                    evz = al(st, [128, 4], F32, "evz")
                    vrt = al(st, [128, 4, 65], F32, "vrt")
                    gfs = al(st, [128, 2, PFW], F32, "gfs")
                    vrb = al(st, [128, 2, 260], F32, "vrb")
                    sm = al(st, [128, 16], F32, "sm")
                    pfs = al(st, [128, PFW], F32, "pfs")
                    wmo = al(st, [128, KC, 256], BF16, "wmo")
                    sgo = al(st, [128, 256], F32, "sgo")
                    note_mem()
                    P.dma("pool", out=wmo[:], in_=wv[:, :, C_MO:C_MO + 256], W=["wmo"])
                    P.op("pool", lambda e: e.memset(Sst[:], 0.0), W=["S0", "S1", "S2"])
                    P.op("pool", lambda e: e.memset(Sbb[:], 0.0), W=["Sb0", "Sb1", "Sb2"])

                    def state_update(si, c, g0, evs, so=None):
                        so = si if so is None else so
                        tc0, _ = chunk_cols(c)
                        for p in range(2):
                            P.op("pe", lambda e: e.transpose(out=psb[:, p * 128:(p + 1) * 128], in_=mqkT[:, 2 + p, tc0:tc0 + 128],
                                                             identity=identb), R=MQ + ["cst"], W=["psb"])
                        P.op("dve", lambda e: e.tensor_tensor(out=k2[:], in0=psb[:, 0:256].rearrange("p (h d) -> p h d", d=64),
                                                              in1=evs.unsqueeze(2).to_broadcast([128, 4, 64]), op=ALU.mult),
                             R=["psb", "ev", "evz"], W=["k2"])
                        for p in range(2):
                            P.op("pe", lambda e: e.matmul(ps[4][:, p * 130:(p + 1) * 130],
                                                          lhsT=k2[:, 2 * p:2 * p + 2, :].rearrange("p h d -> p (h d)"),
                                                          rhs=mv_aug[:, c, 2 * p:2 * p + 2, :].rearrange("p h d -> p (h d)"),
                                                          start=True, stop=True), R=["k2", ("mv", c), "mv_ones"], W=[("ps", 4)])
                        for h in range(4):
                            p = h // 2
                            r0 = (h % 2) * 64
                            c0 = (h % 2) * 65
                            P.op("dve", lambda e: e.scalar_tensor_tensor(
                                out=Sst[r0:r0 + 64, so, p, c0:c0 + 65], in0=Sst[r0:r0 + 64, si, p, c0:c0 + 65],
                                scalar=edec[r0:r0 + 64, c, g0 + h:g0 + h + 1],
                                in1=ps[4][r0:r0 + 64, p * 130 + c0:p * 130 + c0 + 65],
                                op0=ALU.mult, op1=ALU.add), R=[f"S{si}", "edec", ("ps", 4)], W=[f"S{so}"])
                        P.op("act", lambda e: e.activation(out=Sbb[:, so, :, :], in_=Sst[:, so, :, :], func=AF.Identity), R=[f"S{so}"], W=[f"Sb{so}"])

                    def scan_chunk(si, c, dirn):
                        g0 = 0 if dirn == 0 else 4
                        mask = triA if dirn == 0 else triB
                        tc0, _ = chunk_cols(c)
                        P.op("dve", lambda e: e.tensor_tensor(out=Dg[:], in0=identf.unsqueeze(1).to_broadcast([128, 4, 128]),
                                                              in1=eq[:, c, g0:g0 + 4].unsqueeze(2).to_broadcast([128, 4, 128]),
                                                              op=ALU.mult), R=["cst", "eq"], W=["Dg"])
                        P.op("pe", lambda e: e.matmul(ps[0][:, 0:512], lhsT=onesf, rhs=Dg[:].rearrange("p h t -> p (h t)"),
                                                      start=True, stop=True), R=["Dg", "cst"], W=[("ps", 0)])
                        P.op("dve", lambda e: e.tensor_tensor(out=Wt[:], in0=ps[0][:, 0:512].rearrange("p (h t) -> p h t", h=4),
                                                              in1=mask.unsqueeze(1).to_broadcast([128, 4, 128]), op=ALU.mult),
                             R=[("ps", 0), "cst"], W=["Wt"])
                        for h in range(4):
                            b0 = (h % 2) * 64
                            sbk = 1 if h % 2 == 0 else 5
                            P.op("pe", lambda e: e.matmul(ps[sbk][:, (h // 2) * 128:(h // 2 + 1) * 128],
                                                          lhsT=mqkT[b0:b0 + 64, 2 + h // 2, tc0:tc0 + 128],
                                                          rhs=mqkT[b0:b0 + 64, h // 2, tc0:tc0 + 128], start=True, stop=True),
                                 R=MQ, W=[("ps", sbk)])
                        for h in range(4):
                            sbk = 1 if h % 2 == 0 else 5
                            P.op("dve", lambda e: e.scalar_tensor_tensor(out=AT[:, h, :], in0=ps[sbk][:, (h // 2) * 128:(h // 2 + 1) * 128],
                                                                         scalar=ek[:, c, g0 + h:g0 + h + 1], in1=Wt[:, h, :],
                                                                         op0=ALU.mult, op1=ALU.mult),
                                 R=[("ps", sbk), "ek", "Wt"], W=["AT"])
                        for h in range(4):
                            P.op("pe", lambda e: e.matmul(ps[2][:, h * 65:(h + 1) * 65], lhsT=AT[:, h, :], rhs=mv_aug[:, c, h, :],
                                                          start=True, stop=True), R=["AT", ("mv", c), "mv_ones"], W=[("ps", 2)])
                        for p in range(2):
                            P.op("pe", lambda e: e.matmul(ps[3][:, p * 130:(p + 1) * 130], lhsT=mqkT[:, p, tc0:tc0 + 128],
                                                          rhs=Sbb[:, si, p, :], start=True, stop=True), R=MQ + [f"Sb{si}"], W=[("ps", 3)])
                        P.op("act", lambda e: e.activation(out=Nsb[:], in_=ps[2][:, 0:260].rearrange("p (h d) -> p h d", d=65), func=AF.Identity),
                             R=[("ps", 2)], W=["Nsb"])
                        P.op("dve", lambda e: e.tensor_tensor(out=tmpI[:], in0=ps[3][:, 0:260].rearrange("p (h d) -> p h d", d=65),
                                                              in1=eq[:, c, g0:g0 + 4].unsqueeze(2).to_broadcast([128, 4, 65]), op=ALU.mult),
                             R=[("ps", 3), "eq"], W=["tmpI"])
                        P.op("dve", lambda e: e.tensor_tensor(out=Nsb[:], in0=Nsb[:], in1=tmpI[:], op=ALU.add), R=["Nsb", "tmpI"], W=["Nsb"])
                        P.op("dve", lambda e: e.scalar_tensor_tensor(out=dmx[:, 0:4], in0=Nsb[:, :, 64], scalar=-1.0, in1=Nsb[:, :, 64],
                                                                     op0=ALU.mult, op1=ALU.max), R=["Nsb"], W=["dmx"])
                        P.op("dve", lambda e: e.tensor_scalar(out=dmx[:, 0:4], in0=dmx[:, 0:4], scalar1=1.0, scalar2=None, op0=ALU.max),
                             R=["dmx"], W=["dmx"])
                        P.op("dve", lambda e: e.reciprocal(out=dmx[:, 4:8], in_=dmx[:, 0:4]), R=["dmx"], W=["dmx"])
                        hv = hsum[:, c, :].rearrange("p (h d) -> p h d", d=64)
                        if dirn == 0:
                            P.op("dve", lambda e: e.tensor_tensor(out=hv, in0=Nsb[:, :, 0:64],
                                                                  in1=dmx[:, 4:8].unsqueeze(2).to_broadcast([128, 4, 64]), op=ALU.mult),
                                 R=["Nsb", "dmx"], W=[("hsum", c)])
                        else:
                            P.op("dve", lambda e: e.tensor_tensor(out=htmp[:], in0=Nsb[:, :, 0:64],
                                                                  in1=dmx[:, 4:8].unsqueeze(2).to_broadcast([128, 4, 64]), op=ALU.mult),
                                 R=["Nsb", "dmx"], W=["htmp"])
                            P.op("dve", lambda e: e.tensor_tensor(out=hv, in0=hv, in1=htmp[:], op=ALU.add),
                                 R=["htmp", ("hsum", c)], W=[("hsum", c)])
                        state_update(si, c, g0, ev[:, c, g0:g0 + 4])

                    for c in (16, 17) + tuple(range(15)):
                        scan_chunk(0, c, 0)
                        if cfg.get('stop_at') == 'scan1':
                            P.barrier()
                            return True
                    if cfg.get('stop_at') == 'scanA':
                        P.barrier()
                        return True
                    P.op("dve", lambda e: e.tensor_scalar(out=evz[:], in0=ev[:, 15, 0:4], scalar1=notlast, scalar2=None, op0=ALU.mult),
                         R=["ev", "cst"], W=["evz"])
                    state_update(0, 15, 0, evz[:], so=2)
                    P.op("dve", lambda e: e.tensor_tensor(out=vrt[:], in0=mv_aug[:, 15, :, :],
                                                          in1=ev[:, 15, 0:4].unsqueeze(2).to_broadcast([128, 4, 65]), op=ALU.mult),
                         R=[("mv", 15), "mv_ones", "ev"], W=["vrt"])
                    P.op("dve", lambda e: e.tensor_copy(out=pfs[:, 0:4], in_=cb_own[:]), R=["cb_own"], W=["pfs"])
                    P.op("dve", lambda e: e.tensor_copy(out=pfs[:, 4:264], in_=Sst[:, 2, :, :].rearrange("p a b -> p (a b)")), R=["S2"], W=["pfs"])
                    P.op("dve", lambda e: e.tensor_copy(out=pfs[:, 264:266], in_=prelast[:, 2:4]), R=["prelast"], W=["pfs"])
                    P.op("dve", lambda e: e.tensor_copy(out=pfs[:, 266:526], in_=vrt[:].rearrange("p a b -> p (a b)")), R=["vrt"], W=["pfs"])
                    P.dma("sp", out=pf_out[l], in_=pfs[:], R=["pfs"], W=["pf_out"])
                    if exch != "cc":
                        out_keys.extend(["pb_out", "pf_out"])
                    if not last:
                        for c in (17, 16):
                            scan_chunk(1, c, 1)
                    if stop_after_payload:
                        P.barrier()
                        return True

                    P.barrier()
                    if exch == "cc":
                        RG = [[0, 1], [2, 3], [4, 5], [6, 7]]
                        P.cc(lambda e: e.collective_compute("AllGather", ALU.bypass, replica_groups=RG,
                                                            ins=[pb_out[l].opt()], outs=[gb_raw[l].opt()]),
                             R=["pb_out"], W=["gb"])
                        P.cc(lambda e: e.collective_compute("AllGather", ALU.bypass, replica_groups=RG,
                                                            ins=[pf_out[l].opt()], outs=[gf_raw[l].opt()]),
                             R=["pf_out"], W=["gf"])
                    P.dma("sp", out=gfs[:], in_=gf_in[l].rearrange("s p w -> p s w"), R=["gf"], W=["gfs"])
                    for s_ in range(2):
                        P.dma("sp", out=vrb[:, s_, :], in_=gf_in[l][s_, 127, 266:526].partition_broadcast(128), R=["gf"], W=["vrb"])

                    def nbsel(out_ap, a0, a1, Rk, Wk):
                        P.op("dve", lambda e: e.tensor_scalar(out=out_ap, in0=a0, scalar1=selv[:, 0:1], scalar2=None, op0=ALU.mult),
                             R=Rk + ["cst"], W=Wk)
                        P.op("dve", lambda e: e.scalar_tensor_tensor(out=out_ap, in0=a1, scalar=selv[:, 1:2], in1=out_ap,
                                                                     op0=ALU.mult, op1=ALU.add), R=Rk + Wk + ["cst"], W=Wk)
                    nbsel(sm[:, 0:4], gfs[:, 0, 0:4], gfs[:, 1, 0:4], ["gfs"], ["sm"])
                    nbsel(sm[:, 4:6], gfs[:, 0, 264:266], gfs[:, 1, 264:266], ["gfs"], ["sm"])
                    nbsel(Sst[:, 1, :, :].rearrange("p a b -> p (a b)"), gfs[:, 0, 4:264], gfs[:, 1, 4:264], ["gfs", "Sb1"], ["S1"])
                    nbsel(vrt[:].rearrange("p a b -> p (a b)"), vrb[:, 0, :], vrb[:, 1, :], ["vrb"], ["vrt"])
                    P.op("dve", lambda e: e.tensor_tensor(out=sm[:, 8:12], in0=prelast[:], in1=sm[:, 0:4], op=ALU.add),
                         R=["prelast", "sm"], W=["sm"])
                    P.op("act", lambda e: e.activation(out=sm[:, 12:16], in_=sm[:, 8:12], func=AF.Silu), R=["sm"], W=["sm"])
                    P.op("dve", lambda e: e.tensor_copy(out=mqkT[:, :, NL - 1:NL], in_=sm[:, 12:16].unsqueeze(2)), R=["sm"], W=MQ)
                    P.op("dve", lambda e: e.tensor_tensor(out=sm[:, 6:8], in0=sm[:, 4:6], in1=cb_own[:, 2:4], op=ALU.add),
                         R=["sm", "cb_own"], W=["sm"])
                    P.op("act", lambda e: e.activation(out=sm[:, 6:8], in_=sm[:, 6:8], func=AF.Silu), R=["sm"], W=["sm"])
                    for h in range(4):
                        p = h // 2
                        r0 = (h % 2) * 64
                        c0 = (h % 2) * 65
                        P.op("dve", lambda e: e.scalar_tensor_tensor(
                            out=Sst[r0:r0 + 64, 1, p, c0:c0 + 65], in0=vrt[r0:r0 + 64, h, :], scalar=sm[r0:r0 + 64, 6 + p:7 + p],
                            in1=Sst[r0:r0 + 64, 1, p, c0:c0 + 65], op0=ALU.mult, op1=ALU.add), R=["vrt", "sm", "S1"], W=["S1"])
                    P.op("act", lambda e: e.activation(out=Sbb[:, 1, :, :], in_=Sst[:, 1, :, :], func=AF.Identity), R=["S1"], W=["Sb1"])
                    scan_chunk(0, 15, 0)
                    for c in range(15, -1, -1):
                        scan_chunk(1, c, 1)
                    dump(f"hsum{l}", hsum[:], [128, NCH, 256], F32, R=[("hsum", c) for c in range(NCH)])
                    for c in range(NCH):
                        if last and c >= 16:
                            continue
                        tc0, hc0 = chunk_cols(c)
                        pa = ps[5 + c % 2]
                        for kc in range(KC):
                            P.op("pe", lambda e: e.matmul(pa[:, 0:256], lhsT=hxT[:, kc, hc0:hc0 + 128], rhs=wmo[:, kc, :],
                                                          start=(kc == 0), stop=(kc == KC - 1)), R=["wmo"] + HXALL, W=[("ps", 5 + c % 2)])
                        P.op("act", lambda e: e.activation(out=sgo[:], in_=pa[:, 0:256], func=AF.Sigmoid), R=[("ps", 5 + c % 2)], W=["sgo"])
                        P.op("dve", lambda e: e.tensor_tensor(out=AT[:, 0:2, :].rearrange("p a b -> p (a b)"), in0=hsum[:, c, :],
                                                              in1=sgo[:], op=ALU.mult), R=[("hsum", c), "sgo"], W=["AT"])
                        for p in range(2):
                            P.op("pe", lambda e: e.transpose(out=psb[:, p * 128:(p + 1) * 128], in_=AT[:, p, :], identity=identb),
                                 R=["AT", "cst"], W=["psb"])
                        P.op("act", lambda e: e.activation(out=mlT[:, :, tc0:tc0 + 128], in_=psb[:, 0:256].rearrange("p (a t) -> p a t", a=2), func=AF.Identity),
                             R=["psb"], W=["mlT"])
                    P.barrier()
            dump(f"mlT{l}", mlT[:], [128, 2, T], BF16, R=["mlT"])

            with ExitStack() as st_at:
                attT = al(st_at, [128, 4, T], BF16, "attT")
                with ExitStack() as st:
                    watt = al(st, [128, KC, 1408], BF16, "watt")
                    kT = al(st, [128, T], BF16, "kT")
                    V_aug = al(st, [128, NCH, 2, 65], BF16, "Vaug")
                    ropek = al(st, [128, 2, 512], F32, "ropek")
                    ropeq = [al(st, [128, 2, 128], F32, "ropeq") for _ in range(2)]
                    t12 = al(st, [128, 2, 4, 128], F32, "t12")
                    rt = t12[:].rearrange("p a c t -> p a (c t)")
                    qblk = al(st, [128, 2, 4, 128], BF16, "qblk")
                    PT = [al(st, [128, 512], BF16, "PT") for _ in range(7)]
                    att_tok = al(st, [128, 8, 64], BF16, "att_tok")
                    khalo = al(st, [128, 2, 128], BF16, "khalo")
                    vhalo = al(st, [128, 2, 130], BF16, "vhalo")
                    esink = al(st, [128, 8], F32, "esink")
                    dn = al(st, [128, 8], F32, "dn")
                    note_mem()
                    P.dma("pool", out=watt[:], in_=wv[:, :, C_Q:C_Q + 1408], W=["watt"])
                    P.dma("sp", out=khalo[:], in_=gb_in[l][:, :, 4096:4224].rearrange("s p w -> p s w"), R=["gb"], W=["khalo"])
                    P.dma("sp", out=vhalo[:], in_=gb_in[l][:, :, 4224:4354].rearrange("s p w -> p s w"), R=["gb"], W=["vhalo"])
                    P.dma("sp", out=esink[:], in_=sink_d[l].partition_broadcast(128), W=["esink"])
                    P.op("act", lambda e: e.activation(out=esink[:], in_=esink[:], func=AF.Exp), R=["esink"], W=["esink"])
                    P.op("pool", lambda e: e.memset(V_aug[:, :, :, 64:65], 1.0), W=["v_ones"])
                    P.op("pool", lambda e: e.memset(qblk[:], 0.0), W=["qblk"])
                    WQ, WQP, WK, WKP, WV = 0, 512, 1024, 1152, 1280
                    for ti, (xc, hc, n, v) in enumerate(TILES):
                        for kc in range(KC):
                            P.op("pe", lambda e: e.matmul(ps[0][:, 0:n], lhsT=watt[:, kc, WK:WK + 128], rhs=hxT[:, kc, hc:hc + n],
                                                          start=(kc == 0), stop=(kc == KC - 1)), R=["watt"] + HXALL, W=[("ps", 0)])
                        if v == 0:
                            P.dma("sp", out=ropek[:, :, 0:n], in_=rope_d[:, :, xc:xc + n], W=["ropek"])
                            for kc in range(KC):
                                P.op("pe", lambda e: e.matmul(ps[1][:, 0:n], lhsT=watt[:, kc, WKP:WKP + 128], rhs=hxT[:, kc, hc:hc + n],
                                                              start=(kc == 0), stop=(kc == KC - 1)), R=["watt"] + HXALL, W=[("ps", 1)])
                            P.op("dve", lambda e: e.tensor_tensor(out=rt[:, 0, 0:n], in0=ps[0][:, 0:n], in1=ropek[:, 0, 0:n], op=ALU.mult),
                                 R=[("ps", 0), "ropek"], W=["t1"])
                            P.op("dve", lambda e: e.tensor_tensor(out=rt[:, 1, 0:n], in0=ps[1][:, 0:n], in1=ropek[:, 1, 0:n], op=ALU.mult),
                                 R=[("ps", 1), "ropek"], W=["t2"])
                            P.op("dve", lambda e: e.tensor_tensor(out=kT[:, xc:xc + n], in0=rt[:, 0, 0:n], in1=rt[:, 1, 0:n], op=ALU.add),
                                 R=["t1", "t2"], W=["kT"])
                        else:
                            P.op("act", lambda e: e.activation(out=kT[:, xc:xc + n], in_=ps[0][:, 0:n], func=AF.Identity), R=[("ps", 0)], W=["kT"])
                    for c in range(NCH):
                        tc0, hc0 = chunk_cols(c)
                        pa = ps[2 + c % 2]
                        for kc in range(KC):
                            P.op("pe", lambda e: e.matmul(pa[:, 0:128], lhsT=hxT[:, kc, hc0:hc0 + 128], rhs=watt[:, kc, WV:WV + 128],
                                                          start=(kc == 0), stop=(kc == KC - 1)), R=["watt"] + HXALL, W=[("ps", 2 + c % 2)])
                        P.op("dve", lambda e: e.tensor_copy(out=V_aug[:, c, :, 0:64], in_=pa[:, 0:128].rearrange("p (k d) -> p k d", d=64)),
                             R=[("ps", 2 + c % 2), "v_ones"], W=["V"])
                    dump(f"kT{l}", kT[:], [128, T], BF16, R=["kT"])
                    blocks = list(range(16)) + ([] if last else [16, 17])
                    for bi, i in enumerate(blocks):
                        is_ctx = i >= 16
                        tc0, hc0 = chunk_cols(i)
                        for c in range(4):
                            for kc in range(KC):
                                P.op("pe", lambda e: e.matmul(ps[0][:, c * 128:(c + 1) * 128], lhsT=watt[:, kc, WQ + c * 128:WQ + (c + 1) * 128],
                                                              rhs=hxT[:, kc, hc0:hc0 + 128], start=(kc == 0), stop=(kc == KC - 1)),
                                     R=["watt"] + HXALL, W=[("ps", 0)])
                        if not is_ctx:
                            rq = ropeq[bi % 2]
                            P.dma("sp", out=rq[:], in_=rope_d[:, :, tc0:tc0 + 128], W=[("ropeq", bi % 2)])
                            for c in range(4):
                                for kc in range(KC):
                                    P.op("pe", lambda e: e.matmul(ps[1][:, c * 128:(c + 1) * 128],
                                                                  lhsT=watt[:, kc, WQP + c * 128:WQP + (c + 1) * 128],
                                                                  rhs=hxT[:, kc, hc0:hc0 + 128], start=(kc == 0), stop=(kc == KC - 1)),
                                         R=["watt"] + HXALL, W=[("ps", 1)])
                            P.op("dve", lambda e: e.tensor_tensor(out=t12[:, 0, :, :], in0=ps[0][:, 0:512].rearrange("p (c t) -> p c t", c=4),
                                                                  in1=rq[:, 0, :].unsqueeze(1).to_broadcast([128, 4, 128]), op=ALU.mult),
                                 R=[("ps", 0), ("ropeq", bi % 2)], W=["t1"])
                            P.op("dve", lambda e: e.tensor_tensor(out=t12[:, 1, :, :], in0=ps[1][:, 0:512].rearrange("p (c t) -> p c t", c=4),
                                                                  in1=rq[:, 1, :].unsqueeze(1).to_broadcast([128, 4, 128]), op=ALU.mult),
                                 R=[("ps", 1), ("ropeq", bi % 2)], W=["t2"])
                            for kv_ in range(2):
                                P.op("dve", lambda e: e.tensor_tensor(out=qblk[kv_ * 64:(kv_ + 1) * 64, kv_, :, :],
                                                                       in0=t12[kv_ * 64:(kv_ + 1) * 64, 0, :, :],
                                                                       in1=t12[kv_ * 64:(kv_ + 1) * 64, 1, :, :], op=ALU.add),
                                     R=["t1", "t2"], W=["qblk"])
                        else:
                            for kv_ in range(2):
                                P.op("dve", lambda e: e.tensor_copy(out=qblk[kv_ * 64:(kv_ + 1) * 64, kv_, :, :],
                                                                    in_=ps[0][kv_ * 64:(kv_ + 1) * 64, 0:512].rearrange("p (c t) -> p c t", c=4)),
                                     R=[("ps", 0)], W=["qblk"])
                        for kvh in range(2):
                            b0 = kvh * 64
                            klist = []
                            if not is_ctx:
                                if i > 0:
                                    klist.append((kT[:, 128 * (i - 1):128 * i], m_prev, V_aug[:, i - 1, kvh, :]))
                                klist.append((kT[:, 128 * i:128 * (i + 1)], None, V_aug[:, i, kvh, :]))
                                if i < 15:
                                    klist.append((kT[:, 128 * (i + 1):128 * (i + 2)], m_next, V_aug[:, i + 1, kvh, :]))
                                else:
                                    for s_ in range(2):
                                        klist.append((khalo[:, s_, :], mh[s_], vhalo[:, s_, kvh * 65:(kvh + 1) * 65]))
                            for cc in (16, 17):
                                tcc, _ = chunk_cols(cc)
                                klist.append((kT[:, tcc:tcc + 128], None, V_aug[:, cc, kvh, :]))
                            ob = ps[4 + kvh]
                            nk = len(klist)
                            for idx, (kap, mask, vap) in enumerate(klist):
                                spb = 2 + idx % 2
                                P.op("pe", lambda e: e.matmul(ps[spb][:, 0:512], lhsT=kap, rhs=qblk[:, kvh, :, :], start=True, stop=True),
                                     R=["kT", "khalo", "qblk"], W=[("ps", spb)])
                                ptb = PT[idx]
                                P.op("act", lambda e: e.activation(out=ptb[:], in_=ps[spb][:, 0:512], func=AF.Exp, scale=0.125),
                                     R=[("ps", spb)], W=[("PT", idx)])
                                if mask is not None:
                                    P.op("dve", lambda e: e.tensor_tensor(out=ptb[:].rearrange("p (c t) -> p c t", c=4),
                                                                           in0=ptb[:].rearrange("p (c t) -> p c t", c=4),
                                                                           in1=mask.unsqueeze(1).to_broadcast([128, 4, 128]), op=ALU.mult),
                                         R=[("PT", idx), "cst"], W=[("PT", idx)])
                            for j in range(4):
                                for idx, (kap, mask, vap) in enumerate(klist):
                                    P.op("pe", lambda e: e.matmul(ob[:, j * 65:(j + 1) * 65], lhsT=PT[idx][:, j * 128:(j + 1) * 128], rhs=vap,
                                                                  start=(idx == 0), stop=(idx == nk - 1)),
                                         R=[("PT", idx), "V", "vhalo", "v_ones"], W=[("ps", 4 + kvh)])
                            ov = ob[:, 0:260].rearrange("p (h d) -> p h d", d=65)
                            P.op("dve", lambda e: e.tensor_tensor(out=dn[:, 4 * kvh:4 * kvh + 4], in0=ov[:, :, 64],
                                                                  in1=esink[:, 4 * kvh:4 * kvh + 4], op=ALU.add),
                                 R=[("ps", 4 + kvh), "esink"], W=[("dn", kvh)])
                            P.op("dve", lambda e: e.reciprocal(out=dn[:, 4 * kvh:4 * kvh + 4], in_=dn[:, 4 * kvh:4 * kvh + 4]),
                                 R=[("dn", kvh)], W=[("dn", kvh)])
                            P.op("dve", lambda e: e.tensor_tensor(out=att_tok[:, 4 * kvh:4 * kvh + 4, :], in0=ov[:, :, 0:64],
                                                                  in1=dn[:, 4 * kvh:4 * kvh + 4].unsqueeze(2).to_broadcast([128, 4, 64]),
                                                                  op=ALU.mult), R=[("ps", 4 + kvh), ("dn", kvh)], W=["att_tok"])
                        for cc in range(4):
                            P.op("pe", lambda e: e.transpose(out=psb[:, cc * 128:(cc + 1) * 128],
                                                             in_=att_tok[:, 2 * cc:2 * cc + 2, :].rearrange("p h d -> p (h d)"),
                                                             identity=identb), R=["att_tok", "cst"], W=["psb"])
                        P.op("act", lambda e: e.activation(out=attT[:, :, tc0:tc0 + 128], in_=psb[:, 0:512].rearrange("p (c t) -> p c t", c=4), func=AF.Identity),
                             R=["psb"], W=["attT"])
                    P.barrier()
                dump(f"attT{l}", attT[:], [128, 4, T], BF16, R=["attT"])

                with ExitStack() as st_f:
                    yfT = al(st_f, [128, 2, T], BF16, "yfT")
                    with ExitStack() as st:
                        ufg = al(st, [128, 2, 2, NL], BF16, "ufg")
                        wbd = al(st, [128, 2, 128], F32, "wbd")
                        Gbd = al(st, [128, 2, 256], BF16, "Gbd")
                        PQb = [al(st, [128, 512], BF16, "PQb") for _ in range(2)]
                        dftb = [al(st, [128, 2, 2, 512], BF16, "dftb") for _ in range(3)]
                        dftc = al(st, [128, 2, 2, 256], BF16, "dftc")
                        note_mem()
                        for s_ in range(2):
                            P.dma("sp", out=ufg[:, s_, :, :], in_=gb_in[l][s_, :, 0:4096].rearrange("p (c t) -> p c t", c=2), R=["gb"], W=["ufg"])
                        P.op("pool", lambda e: e.memset(wbd[:], 0.0), W=["wbd"])
                        for g in range(4):
                            r0 = (g % 2) * 64
                            P.dma("sp", out=wbd[r0:r0 + 64, g // 2, r0:r0 + 64], in_=wfour_d[l, g], R=["wbd"], W=[("wbdd", g)])
                        for ch in range(2):
                            for k_ in range(2):
                                P.op("pe", lambda e: e.matmul(ps[6][:, ch * 256 + k_ * 128:ch * 256 + (k_ + 1) * 128],
                                                              lhsT=ccs[:, k_ * 128:(k_ + 1) * 128], rhs=wbd[:, ch, :], start=True, stop=True),
                                     R=["cst", "wbd"] + [("wbdd", g) for g in range(4)], W=[("ps", 6)])
                        P.op("act", lambda e: e.activation(out=Gbd[:], in_=ps[6][:, 0:512].rearrange("p (c w) -> p c w", c=2), func=AF.Identity), R=[("ps", 6)], W=["Gbd"])
                        for pass_ in range(2):
                            for nchunk in range(32):
                                slot, j = divmod(nchunk, 16)
                                pqp = ps[4 + nchunk % 2]
                                for ch in range(2):
                                    P.op("pe", lambda e: e.matmul(pqp[:, ch * 256:(ch + 1) * 256], lhsT=ufg[:, slot, ch, j * 128:(j + 1) * 128],
                                                                  rhs=Gbd[:, ch, :], start=True, stop=True), R=["ufg", "Gbd"], W=[("ps", 4 + nchunk % 2)])
                                pqb = PQb[nchunk % 2]
                                if nchunk % 2 == 0:
                                    P.op("act", lambda e: e.activation(out=pqb[:], in_=pqp[:, 0:512], func=AF.Identity), R=[("ps", 4 + nchunk % 2)], W=[("PQb", nchunk % 2)])
                                else:
                                    P.op("dve", lambda e: e.tensor_copy(out=pqb[:], in_=pqp[:, 0:512]), R=[("ps", 4 + nchunk % 2)], W=[("PQb", nchunk % 2)])
                                db = dftb[nchunk % 3]
                                P.dma("sp", out=db[:], in_=dftl_d[pass_, nchunk], W=[("dftb", nchunk % 3)])
                                for ktl in range(2):
                                    for ec in range(2):
                                        acc = ps[ktl * 2 + ec]
                                        P.op("pe", lambda e: e.matmul(acc[:, 0:512], lhsT=pqb[:, ec * 256:ec * 256 + 128], rhs=db[:, ktl, 0, :],
                                                                      start=(nchunk == 0), stop=False),
                                             R=[("PQb", nchunk % 2), ("dftb", nchunk % 3)], W=[("ps", ktl * 2 + ec)])
                                        P.op("pe", lambda e: e.matmul(acc[:, 0:512], lhsT=pqb[:, ec * 256 + 128:ec * 256 + 256], rhs=db[:, ktl, 1, :],
                                                                      start=False, stop=(nchunk == 31)),
                                             R=[("PQb", nchunk % 2), ("dftb", nchunk % 3)], W=[("ps", ktl * 2 + ec)])
                            for ktl in range(2):
                                for ec in range(2):
                                    k0 = (pass_ * 2 + ktl) * 512
                                    P.op("act", lambda e: e.activation(out=yfT[:, ec, k0:k0 + 512], in_=ps[ktl * 2 + ec][:, 0:512], func=AF.Identity),
                                         R=[("ps", ktl * 2 + ec)], W=["yfT"])
                        if not last:
                            P.dma("sp", out=dftc[:], in_=dftc_d, W=["dftc"])
                            for nck in range(2):
                                pqp = ps[4 + nck]
                                for ch in range(2):
                                    P.op("pe", lambda e: e.matmul(pqp[:, ch * 256:(ch + 1) * 256], lhsT=ufc[:, ch, nck * 128:(nck + 1) * 128],
                                                                  rhs=Gbd[:, ch, :], start=True, stop=True), R=["ufc", "Gbd"], W=[("ps", 4 + nck)])
                                P.op("act", lambda e: e.activation(out=PQb[nck][:], in_=pqp[:, 0:512], func=AF.Identity), R=[("ps", 4 + nck)], W=[("PQb", nck)])
                            for ec in range(2):
                                for nck in range(2):
                                    P.op("pe", lambda e: e.matmul(ps[ec][:, 0:256], lhsT=PQb[nck][:, ec * 256:ec * 256 + 128], rhs=dftc[:, nck, 0, :],
                                                                  start=(nck == 0), stop=False), R=[("PQb", nck), "dftc"], W=[("ps", ec)])
                                    P.op("pe", lambda e: e.matmul(ps[ec][:, 0:256], lhsT=PQb[nck][:, ec * 256 + 128:ec * 256 + 256], rhs=dftc[:, nck, 1, :],
                                                                  start=False, stop=(nck == 1)), R=[("PQb", nck), "dftc"], W=[("ps", ec)])
                                P.op("act", lambda e: e.activation(out=yfT[:, ec, NL:T], in_=ps[ec][:, 0:256], func=AF.Identity), R=[("ps", ec)], W=["yfT"])
                        P.barrier()
                    dump(f"yfT{l}", yfT[:], [128, 2, T], BF16, R=["yfT"])

                    with ExitStack() as st:
                        wo = al(st, [128, KC, D], BF16, "wo")
                        note_mem()
                        P.dma("pool", out=wo[:], in_=wout_d[l].rearrange("(rc p) n -> p rc n", p=128), W=["wo"])
                        srcs = [yfT[:, 0, :], yfT[:, 1, :], attT[:, 0, :], attT[:, 1, :], attT[:, 2, :], attT[:, 3, :], mlT[:, 0, :], mlT[:, 1, :]]
                        it = 0
                        for oc in range(KC):
                            for ti in (range(5) if not last else range(4)):
                                xc, hc, n, v = TILES[ti]
                                pbk = it % 4
                                it += 1
                                for rc in range(KC):
                                    P.op("pe", lambda e: e.matmul(ps[pbk][:, 0:n], lhsT=wo[:, rc, oc * 128:(oc + 1) * 128], rhs=srcs[rc][:, xc:xc + n],
                                                                  start=(rc == 0), stop=(rc == KC - 1)),
                                         R=["wo", "yfT", "attT", "mlT"], W=[("ps", pbk)])
                                P.op("dve", lambda e: e.scalar_tensor_tensor(out=xT[:, oc, xc:xc + n], in0=ps[pbk][:, 0:n],
                                                                             scalar=g_ap(1, v, oc), in1=xT[:, oc, xc:xc + n],
                                                                             op0=ALU.mult, op1=ALU.add),
                                     R=[("ps", pbk), ("x", ti), "G_t"], W=[("x", ti)])
                        P.barrier()
        return False

    nlayers = cfg.get("layers", DEPTH)
    stop_payload = cfg.get("stop_payload", False)
    stopped = False
    for l in range(nlayers):
        last = (l == DEPTH - 1)
        mods_phase(l)
        dump(f"modT{l}", modT[:], [128, 72, 2], R=["modT"])
        if not cfg.get('skip_ffn'):
            ffn_phase(l, 0, 0, range(5))
        dump(f"x_ffn0_{l}", xT[:], [128, KC, T], R=[("x", i) for i in range(5)])
        if cfg.get("mixer", True):
            stopped = mixer_phase(l, stop_payload and l == nlayers - 1)
            if stopped:
                break
            dump(f"x_mix_{l}", xT[:], [128, KC, T], R=[("x", i) for i in range(5)])
        ffn_phase(l, 1, 2, range(5) if not last else range(4))
        dump(f"x_ffn1_{l}", xT[:], [128, KC, T], R=[("x", i) for i in range(5)])
    if nlayers == DEPTH and not stopped:
        final_phase()
    else:
        P.finish(out_keys)
        P.barrier()
    print("min sbuf bytes remaining:", minrem[0], "instr counts:", P.cnt)
    return nc


def _fm(a):
    t = a.shape[0]
    return np.ascontiguousarray(a.reshape(t, KC, 128).transpose(2, 1, 0))


def host_shared(inp):
    sh = {}
    sh["w_ada"] = np.ascontiguousarray(inp["w_ada"], dtype=np.float32)
    sh["b_adaT"] = np.ascontiguousarray(inp["b_ada"].reshape(DEPTH, 72, 128).transpose(2, 0, 1))
    sh["norm_gT"] = np.ascontiguousarray(inp["norm_g"].reshape(DEPTH, 3, KC, 128).transpose(3, 0, 1, 2))
    sh["g_finT"] = np.ascontiguousarray(inp["g_final"].reshape(KC, 128).T)
    w1 = inp["w_ffn_in"]
    g = w1[..., :DFF].reshape(DEPTH, 2, KC, 128, NJ, 128)
    u = w1[..., DFF:].reshape(DEPTH, 2, KC, 128, NJ, 128)
    gu = np.concatenate([g, u], axis=-1)
    sh["W1r"] = np.ascontiguousarray(gu.transpose(0, 1, 4, 3, 2, 5))
    sh["W2"] = np.ascontiguousarray(inp["w_ffn_out"], dtype=np.float32)
    return sh


def host_core(inp, b, h):
    flip = (h == 1)
    xs = inp["x"][b, h * NL:(h + 1) * NL]
    cx = inp["ctx"][b]
    if flip:
        xs = xs[::-1]
        cx = cx[::-1]
    m = {}
    m["xT0"] = _fm(np.concatenate([xs, cx], axis=0))
    m["cT"] = np.ascontiguousarray(np.stack([inp["c"][b], inp["c_ctx"]], axis=-1).reshape(KC, 128, 2).transpose(1, 0, 2))
    return m


_BF = ml_dtypes.bfloat16
_PERM = np.concatenate([np.arange(16, 32), np.arange(0, 16), np.arange(48, 64), np.arange(32, 48)])
_SIGN = np.concatenate([-np.ones(16), np.ones(16), -np.ones(16), np.ones(16)]).astype(np.float32)


def host_consts(parity):
    c = {}
    r = np.arange(128)
    cf = np.zeros((128, 1024), np.float32)
    cf[:, 0:128] = np.eye(128)
    cf[:, 128:256] = (r[:, None] <= r[None, :])
    cf[:, 256:384] = (r[:, None] >= r[None, :])
    cf[:, 384:512] = 1.0
    k64 = np.arange(64)
    ang = 2 * np.pi * ((k64[:, None] * k64[None, :]) % 64) / 64.0
    Cc = np.cos(ang) / 8.0
    Sc = np.sin(ang) / 8.0
    cf[0:64, 512:576] = Cc
    cf[64:128, 576:640] = Cc
    cf[0:64, 640:704] = Sc
    cf[64:128, 704:768] = Sc
    cf[:, 768] = 0.0 if parity == 0 else 1.0
    cf[:, 769] = 1.0 if parity == 0 else 0.0
    cf[:, 770] = 1.0
    cf[127, 770] = 0.0
    c["cst_f"] = cf
    cb = np.zeros((128, 640), np.float32)
    cb[:, 0:128] = np.eye(128)
    cb[:, 128:256] = (r[:, None] >= r[None, :])
    cb[:, 256:384] = (r[:, None] <= r[None, :])
    hm = ((r[:, None] + r[None, :]) >= 127).astype(np.float32)
    if parity == 0:
        cb[:, 512:640] = hm
    else:
        cb[:, 384:512] = hm
    c["cst_b"] = cb.astype(_BF)
    t = np.arange(NL)
    g = t if parity == 0 else (2 * NL - 1 - t)
    row = (g // 64).astype(np.float32)
    col = (g % 64).astype(np.float32)
    inv = (np.float32(10000.0) ** (-(np.arange(16, dtype=np.float32) / np.float32(16)))).astype(np.float32)
    ar = row[:, None] * inv[None, :]
    ac = col[:, None] * inv[None, :]
    angr = np.concatenate([ar, ar, ac, ac], axis=-1).astype(np.float32)
    cosT = np.cos(angr).T.astype(np.float32)
    sinT = (np.sin(angr).T * _SIGN[:, None]).astype(np.float32)
    rope = np.zeros((128, 2, NL), np.float32)
    rope[0:64, 0] = cosT
    rope[64:128, 0] = cosT
    rope[0:64, 1] = sinT
    rope[64:128, 1] = sinT
    c["rope"] = rope
    N = 2 * NL
    tabc = np.cos(2 * np.pi * np.arange(N) / N) / 64.0
    tabs = -np.sin(2 * np.pi * np.arange(N) / N) / 64.0
    j = np.arange(NL)
    nglob = np.concatenate([j, N - 1 - j])
    kl = np.arange(NL)
    kglob = kl if parity == 0 else (N - 1 - kl)
    idx = (nglob[:, None].astype(np.int64) * kglob[None, :].astype(np.int64)) % N
    dl = np.empty((2, 32, 128, 2, 2, 512), _BF)
    ic = tabc[idx].reshape(32, 128, 2, 2, 512)
    isn = tabs[idx].reshape(32, 128, 2, 2, 512)
    dl[:, :, :, :, 0, :] = ic.transpose(2, 0, 1, 3, 4).astype(_BF)
    dl[:, :, :, :, 1, :] = isn.transpose(2, 0, 1, 3, 4).astype(_BF)
    c["dft_lat"] = dl
    nc_ = np.arange(NCX)
    ng = nc_ if parity == 0 else (NCX - 1 - nc_)
    idc = (ng[:, None] * ng[None, :]) % NCX
    tc = np.cos(2 * np.pi * np.arange(NCX) / NCX) / 16.0
    ts = -np.sin(2 * np.pi * np.arange(NCX) / NCX) / 16.0
    dc = np.empty((128, 2, 2, 256), _BF)
    dc[:, :, 0, :] = tc[idc].reshape(2, 128, 256).transpose(1, 0, 2).astype(_BF)
    dc[:, :, 1, :] = ts[idc].reshape(2, 128, 256).transpose(1, 0, 2).astype(_BF)
    c["dft_ctx"] = dc
    return c


def host_parity(inp, parity):
    m = {}
    w = inp["w_in"]
    L = w.shape[0]
    wp = np.empty((L, D, NCOL), np.float32)
    wp[:, :, C_F:C_F + 256] = w[:, :, 0:256]
    q = w[:, :, 256:768].reshape(L, D, 8, 64)
    qperm = q[:, :, :, _PERM]
    for c in range(4):
        wp[:, :, C_Q + c * 128:C_Q + c * 128 + 64] = q[:, :, c]
        wp[:, :, C_Q + c * 128 + 64:C_Q + (c + 1) * 128] = q[:, :, c + 4]
        wp[:, :, C_QP + c * 128:C_QP + c * 128 + 64] = qperm[:, :, c]
        wp[:, :, C_QP + c * 128 + 64:C_QP + (c + 1) * 128] = qperm[:, :, c + 4]
    k = w[:, :, 768:896].reshape(L, D, 2, 64)
    wp[:, :, C_K:C_K + 128] = k.reshape(L, D, 128)
    wp[:, :, C_KP:C_KP + 128] = k[:, :, :, _PERM].reshape(L, D, 128)
    wp[:, :, C_V:C_V + 128] = w[:, :, 896:1024]
    wp[:, :, C_MV:C_MV + 256] = w[:, :, 1536:1792]
    mi = w[:, :, 2048:2056].reshape(L, D, 2, 4)
    mf = w[:, :, 2056:2064].reshape(L, D, 2, 4)
    if parity == 1:
        mi = mi[:, :, ::-1]
        mf = mf[:, :, ::-1]
    wp[:, :, C_G:C_G + 8] = mi.reshape(L, D, 8)
    wp[:, :, C_G + 8:C_G + 16] = mf.reshape(L, D, 8)
    wp[:, :, C_MO:C_MO + 256] = w[:, :, 1792:2048]
    wp[:, :, C_MQK:C_MQK + 512] = w[:, :, 1024:1536]
    m["w_in_p"] = wp
    cq = inp["conv_qk"]
    if parity == 1:
        cq = cq[:, ::-1]
    m["convT"] = np.ascontiguousarray(cq.reshape(L, 3, 4, 128).transpose(3, 0, 2, 1))
    bi = inp["b_gate_i"]
    bf = inp["b_gate_f"]
    if parity == 1:
        bi = bi[:, ::-1]
        bf = bf[:, ::-1]
    m["bgate"] = np.ascontiguousarray(np.concatenate([bi.reshape(L, 8), bf.reshape(L, 8)], axis=1))
    return m


def host_all(inp):
    inp = {k: np.asarray(v, dtype=np.float32) for k, v in inp.items()}
    sh = host_shared(inp)
    sh["sink"] = np.ascontiguousarray(inp["attn_sink"])
    sh["w_fourier"] = np.ascontiguousarray(inp["w_fourier"])
    sh["w_out"] = np.ascontiguousarray(inp["w_out"])
    par = []
    for p in range(2):
        d = dict(host_consts(p))
        d.update(host_parity(inp, p))
        par.append(d)
    maps = []
    for b in range(4):
        for h in range(2):
            m = dict(sh)
            m.update(par[h])
            m.update(host_core(inp, b, h))
            for l in range(DEPTH):
                m[f"gb_in{l}"] = np.zeros((2, 128, PBW), _BF)
                m[f"gf_in{l}"] = np.zeros((2, 128, PFW), np.float32)
            maps.append(m)
    return maps


def _exchange(maps, res, l):
    for b in range(4):
        pb = np.stack([res.results[2 * b][f"pb_out{l}"], res.results[2 * b + 1][f"pb_out{l}"]])
        pf = np.stack([res.results[2 * b][f"pf_out{l}"], res.results[2 * b + 1][f"pf_out{l}"]])
        for h in range(2):
            maps[2 * b + h][f"gb_in{l}"] = pb
            maps[2 * b + h][f"gf_in{l}"] = pf


_NC_CACHE = {}


def _get_nc(key, cfg):
    if key not in _NC_CACHE:
        _NC_CACHE[key] = build(cfg)
    return _NC_CACHE[key]


def kernel(**inputs):
    maps = host_all(inputs)
    cores = list(range(8))
    for m in maps:
        for l in range(DEPTH):
            m.pop(f"gb_in{l}", None)
            m.pop(f"gf_in{l}", None)
    res = run_bass_kernel_spmd(build(dict(layers=2, exch="cc")), maps, core_ids=cores)
    out = np.empty((4, 2 * NL, D), np.float32)
    for b in range(4):
        for h in range(2):
            o = res.results[2 * b + h]["outT"]
            tok = o.transpose(2, 1, 0).reshape(NL, D)
            if h == 1:
                tok = tok[::-1]
            out[b, h * NL:(h + 1) * NL] = tok
    return out
```
